# Optimizing a Trainium2 kernel written in Bass

```python
import jax, jax.numpy as jnp
from jax import lax
import numpy as np

D_MODEL = 2048
BATCH = 8
SEQ = 2048
DEPTH = 4

D_MIX = D_MODEL
CONV_CH = D_MIX // 4
CONV_GROUPS = 4
CONV_WIDTH = 31
ML_HEADS = 4
ML_DIM = D_MIX // 4
ML_HEAD_DIM = ML_DIM // ML_HEADS
ML_CONV_WIDTH = 4
ML_CHUNK = 128
ATT_DIM = D_MIX - CONV_CH - ML_DIM
ATT_HEADS = 8
ATT_HEAD_DIM = ATT_DIM // ATT_HEADS
MOBA_BLOCK = 256
MOBA_TOPK = 3
MOBA_Q_CHUNK = 16
D_IN = 2 * CONV_CH + 4 * ML_DIM + 2 * ML_HEADS + 3 * ATT_DIM
D_FF = 5632
N_EXPERTS = 8
TOP_K = 2
N_DENSE = (DEPTH + 1) // 2
N_MOE = DEPTH // 2
EPS = 1e-6
NEG = -1e30

kernel_name = 'hymba_style_conv_mlstm_moba_moe_trunk'


def rmsnorm(x, g):
    xf = x.astype(jnp.float32)
    y = xf * lax.rsqrt(jnp.mean(xf * xf, axis=-1, keepdims=True) + EPS)
    return (y * g.astype(jnp.float32)).astype(x.dtype)


def modulate(h, shift, scale):
    return h * (1 + scale[:, None, :]) + shift[:, None, :]


def causal_dwconv(u, w, b):
    k = w.shape[0]
    out = lax.conv_general_dilated(u, w[:, None, :].astype(u.dtype), window_strides=(1,), padding=[(k - 1, 0)], dimension_numbers=('NWC', 'WIO', 'NWC'), feature_group_count=u.shape[-1])
    return out + b.astype(u.dtype)


def conv_module(a, g, w, b, ln_g, ln_b):
    u = causal_dwconv(a * jax.nn.sigmoid(g), w, b)
    B, T, C = u.shape
    uf = u.astype(jnp.float32).reshape(B, T, CONV_GROUPS, C // CONV_GROUPS)
    mu = jnp.mean(uf, axis=-1, keepdims=True)
    var = jnp.mean(jnp.square(uf - mu), axis=-1, keepdims=True)
    uf = ((uf - mu) * lax.rsqrt(var + EPS)).reshape(B, T, C) * ln_g + ln_b
    return jax.nn.silu(uf).astype(a.dtype)


def mlstm_chunk(carry, xs):
    C, n, m = carry
    q, k, v, log_i, log_f = xs
    L = q.shape[2]
    b = jnp.cumsum(log_f, axis=-1)
    causal = jnp.tril(jnp.ones((L, L), dtype=bool))
    D = jnp.where(causal, b[..., :, None] - b[..., None, :] + log_i[..., None, :], NEG)
    inter = b + m[..., None]
    m_t = jnp.maximum(jnp.max(D, axis=-1), inter)
    S = jnp.einsum('bhtd,bhsd->bhts', q, k) * jnp.exp(D - m_t[..., None])
    w_inter = jnp.exp(inter - m_t)
    num = jnp.einsum('bhts,bhsd->bhtd', S, v) + w_inter[..., None] * jnp.einsum('bhvk,bhtk->bhtv', C, q)
    den = jnp.sum(S, axis=-1) + w_inter * jnp.einsum('bhk,bhtk->bht', n, q)
    h = num / jnp.maximum(jnp.abs(den), jnp.exp(-m_t))[..., None]
    b_L = b[..., -1]
    log_w = b_L[..., None] - b + log_i
    m_new = jnp.maximum(b_L + m, jnp.max(log_w, axis=-1))
    wk = jnp.exp(log_w - m_new[..., None])
    decay = jnp.exp(b_L + m - m_new)
    C_new = decay[..., None, None] * C + jnp.einsum('bhs,bhsv,bhsk->bhvk', wk, v, k)
    n_new = decay[..., None] * n + jnp.einsum('bhs,bhsk->bhk', wk, k)
    return (C_new, n_new, m_new), h


def mlstm(q, k, v, i_pre, f_pre, b_i, b_f, norm_g):
    B, T, _ = q.shape
    H, dh, L = ML_HEADS, ML_HEAD_DIM, ML_CHUNK
    NC = T // L
    def heads(t):
        return t.astype(jnp.float32).reshape(B, T, H, dh).transpose(0, 2, 1, 3).reshape(B, H, NC, L, dh).transpose(2, 0, 1, 3, 4)
    def gates(t):
        return t.astype(jnp.float32).transpose(0, 2, 1).reshape(B, H, NC, L).transpose(2, 0, 1, 3)
    log_i = gates(i_pre + b_i)
    log_f = jax.nn.log_sigmoid(gates(f_pre + b_f))
    init = (jnp.zeros((B, H, dh, dh), jnp.float32), jnp.zeros((B, H, dh), jnp.float32), jnp.zeros((B, H), jnp.float32))
    _, hs = lax.scan(mlstm_chunk, init, (heads(q), heads(k) * (dh ** -0.5), heads(v), log_i, log_f))
    hs = hs.transpose(1, 2, 0, 3, 4).reshape(B, H, T, dh).transpose(0, 2, 1, 3)
    hs = hs * lax.rsqrt(jnp.mean(hs * hs, axis=-1, keepdims=True) + EPS) * norm_g.astype(jnp.float32).reshape(H, dh)
    return hs.reshape(B, T, H * dh)


def moba_attention(q, k, v, norm_g):
    B, T, _ = q.shape
    H, dh, BLK, QC = ATT_HEADS, ATT_HEAD_DIM, MOBA_BLOCK, MOBA_Q_CHUNK
    NB = -(-T // BLK)
    T_pad = NB * BLK
    topk = min(MOBA_TOPK, NB)
    NSEL = topk + 1
    def heads(t):
        t = t.reshape(B, T, H, dh).transpose(0, 2, 1, 3)
        return jnp.pad(t, ((0, 0), (0, 0), (0, T_pad - T), (0, 0)))
    qh = heads(q) * (dh ** -0.5)
    kb = heads(k).reshape(B, H, NB, BLK, dh)
    vb = heads(v).reshape(B, H, NB, BLK, dh)
    pos = jnp.arange(T_pad)
    qblk = pos // BLK
    k_mean = jnp.mean(kb.astype(jnp.float32), axis=3)
    gate = jnp.einsum('bhtd,bhnd->bhtn', qh.astype(jnp.float32), k_mean)
    past = jnp.arange(NB)[None, :] < qblk[:, None]
    gate = jnp.where(past, gate, NEG)
    _, top_idx = lax.top_k(gate, topk)
    own = jnp.broadcast_to(qblk[None, None, :, None], (B, H, T_pad, 1)).astype(top_idx.dtype)
    sel = jnp.concatenate([top_idx, own], axis=-1)
    nq = T_pad // QC
    q_c = qh.reshape(B, H, nq, QC, dh).transpose(2, 0, 1, 3, 4)
    sel_c = sel.reshape(B, H, nq, QC, NSEL).transpose(2, 0, 1, 3, 4)
    pos_c = pos.reshape(nq, QC)
    slopes = 2.0 ** (-(8.0 / H) * jnp.arange(1, H + 1, dtype=jnp.float32))
    slot_routed = jnp.arange(NSEL) < topk
    gather = jax.vmap(jax.vmap(lambda table, ix: table[ix]))
    def attend(args):
        qc, ic, pc = args
        kg = gather(kb, ic)
        vg = gather(vb, ic)
        logits = jnp.einsum('bhqd,bhqnsd->bhqns', qc, kg).astype(jnp.float32)
        kpos = ic[..., None] * BLK + jnp.arange(BLK)
        dist = pc[:, None, None] - kpos
        logits = logits - slopes[:, None, None, None] * jnp.abs(dist).astype(jnp.float32)
        routed_ok = slot_routed & (ic < (pc // BLK)[:, None])
        valid = routed_ok[..., None] | ((~slot_routed)[:, None] & (dist >= 0))
        logits = jnp.where(valid, logits, NEG)
        p = jax.nn.softmax(logits.reshape(B, H, QC, NSEL * BLK), axis=-1).reshape(logits.shape)
        return jnp.einsum('bhqns,bhqnsd->bhqd', p.astype(vg.dtype), vg)
    out = lax.map(attend, (q_c, sel_c, pos_c))
    out = out.transpose(1, 2, 0, 3, 4).reshape(B, H, T_pad, dh)[:, :, :T].transpose(0, 2, 1, 3)
    of = out.astype(jnp.float32)
    of = of * lax.rsqrt(jnp.mean(of * of, axis=-1, keepdims=True) + EPS) * norm_g.astype(jnp.float32).reshape(H, dh)
    return of.reshape(B, T, H * dh).astype(q.dtype)


def hybrid_mixer(h, w_in, conv_w, conv_b, conv_ln_g, conv_ln_b, ml_conv_w, ml_conv_b, ml_b_i, ml_b_f, ml_norm_g, attn_norm_g, w_out):
    z = h @ w_in
    sizes = [CONV_CH, CONV_CH, ML_DIM, ML_DIM, ML_DIM, ML_DIM, ML_HEADS, ML_HEADS, ATT_DIM, ATT_DIM, ATT_DIM]
    ca, cg, mq, mk, mv, mo, mi, mf, aq, ak, av = jnp.split(z, [int(s) for s in np.cumsum(sizes)[:-1]], axis=-1)
    y_conv = conv_module(ca, cg, conv_w, conv_b, conv_ln_g, conv_ln_b)
    qk = jax.nn.silu(causal_dwconv(jnp.concatenate([mq, mk], axis=-1), ml_conv_w, ml_conv_b))
    mq, mk = jnp.split(qk, 2, axis=-1)
    y_ml = (jax.nn.sigmoid(mo.astype(jnp.float32)) * mlstm(mq, mk, mv, mi, mf, ml_b_i, ml_b_f, ml_norm_g)).astype(h.dtype)
    y_att = moba_attention(aq, ak, av, attn_norm_g)
    return jnp.concatenate([y_conv, y_ml, y_att], axis=-1) @ w_out


def swiglu(h, wg, wu, wd):
    return (jax.nn.silu(h @ wg) * (h @ wu)) @ wd


def moe_swiglu(h, w_router, b_router, w_gate, w_up, w_down):
    B, T, D = h.shape
    hf = h.reshape(B * T, D)
    logits = (hf @ w_router + b_router).astype(jnp.float32)
    top_val, top_idx = lax.top_k(logits, TOP_K)
    probs = jax.nn.softmax(top_val, axis=-1)
    gates = jnp.sum(jax.nn.one_hot(top_idx, N_EXPERTS, dtype=jnp.float32) * probs[..., None], axis=1).astype(hf.dtype)
    y = jnp.zeros_like(hf)
    for e in range(N_EXPERTS):
        y = y + gates[:, e:e + 1] * swiglu(hf, w_gate[e], w_up[e], w_down[e])
    return y.reshape(B, T, D)


def setup_inputs(seed: int = 0) -> dict:
    key = jax.random.key(seed)
    ks = jax.random.split(key, 32)
    f32 = jnp.float32
    D = D_MODEL
    def nrm(k, shape, scale):
        return jax.random.normal(k, shape, f32) * scale
    return {
        'x': nrm(ks[0], (BATCH, SEQ, D), 1.0),
        'c': nrm(ks[1], (BATCH, D), 1.0),
        'w_mod': nrm(ks[2], (DEPTH, D, 6 * D), 0.5 * D ** -0.5),
        'b_mod': nrm(ks[3], (DEPTH, 6 * D), 0.01),
        'g_mix': 1.0 + nrm(ks[4], (DEPTH, D), 0.02),
        'g_ffn': 1.0 + nrm(ks[5], (DEPTH, D), 0.02),
        'w_in': nrm(ks[6], (DEPTH, D, D_IN), D ** -0.5),
        'conv_w': nrm(ks[7], (DEPTH, CONV_WIDTH, CONV_CH), CONV_WIDTH ** -0.5),
        'conv_b': nrm(ks[8], (DEPTH, CONV_CH), 0.01),
        'conv_ln_g': 1.0 + nrm(ks[9], (DEPTH, CONV_CH), 0.02),
        'conv_ln_b': nrm(ks[10], (DEPTH, CONV_CH), 0.01),
        'ml_conv_w': nrm(ks[11], (DEPTH, ML_CONV_WIDTH, 2 * ML_DIM), ML_CONV_WIDTH ** -0.5),
        'ml_conv_b': nrm(ks[12], (DEPTH, 2 * ML_DIM), 0.01),
        'ml_b_i': nrm(ks[13], (DEPTH, ML_HEADS), 0.1),
        'ml_b_f': jnp.linspace(3.0, 6.0, ML_HEADS, dtype=f32)[None, :] + nrm(ks[14], (DEPTH, ML_HEADS), 0.1),
        'ml_norm_g': 1.0 + nrm(ks[15], (DEPTH, ML_DIM), 0.02),
        'attn_norm_g': 1.0 + nrm(ks[16], (DEPTH, ATT_DIM), 0.02),
        'w_out': nrm(ks[17], (DEPTH, D_MIX, D), D_MIX ** -0.5),
        'ffn_w_gate': nrm(ks[18], (N_DENSE, D, D_FF), D ** -0.5),
        'ffn_w_up': nrm(ks[19], (N_DENSE, D, D_FF), D ** -0.5),
        'ffn_w_down': nrm(ks[20], (N_DENSE, D_FF, D), D_FF ** -0.5),
        'moe_w_router': nrm(ks[21], (N_MOE, D, N_EXPERTS), D ** -0.5),
        'moe_b_router': nrm(ks[22], (N_MOE, N_EXPERTS), 0.01),
        'moe_w_gate': nrm(ks[23], (N_MOE, N_EXPERTS, D, D_FF), D ** -0.5),
        'moe_w_up': nrm(ks[24], (N_MOE, N_EXPERTS, D, D_FF), D ** -0.5),
        'moe_w_down': nrm(ks[25], (N_MOE, N_EXPERTS, D_FF, D), D_FF ** -0.5),
        'g_final': 1.0 + nrm(ks[26], (D,), 0.02),
    }


def reference(x, c, w_mod, b_mod, g_mix, g_ffn, w_in, conv_w, conv_b, conv_ln_g, conv_ln_b, ml_conv_w, ml_conv_b, ml_b_i, ml_b_f, ml_norm_g, attn_norm_g, w_out, ffn_w_gate, ffn_w_up, ffn_w_down, moe_w_router, moe_b_router, moe_w_gate, moe_w_up, moe_w_down, g_final):
    cond = jax.nn.silu(c)
    for l in range(DEPTH):
        mod = cond @ w_mod[l] + b_mod[l]
        sh1, sc1, gt1, sh2, sc2, gt2 = jnp.split(mod, 6, axis=-1)
        h = modulate(rmsnorm(x, g_mix[l]), sh1, sc1)
        y = hybrid_mixer(h, w_in[l], conv_w[l], conv_b[l], conv_ln_g[l], conv_ln_b[l], ml_conv_w[l], ml_conv_b[l], ml_b_i[l], ml_b_f[l], ml_norm_g[l], attn_norm_g[l], w_out[l])
        x = x + gt1[:, None, :] * y
        h = modulate(rmsnorm(x, g_ffn[l]), sh2, sc2)
        if l % 2 == 0:
            f = swiglu(h, ffn_w_gate[l // 2], ffn_w_up[l // 2], ffn_w_down[l // 2])
        else:
            f = moe_swiglu(h, moe_w_router[l // 2], moe_b_router[l // 2], moe_w_gate[l // 2], moe_w_up[l // 2], moe_w_down[l // 2])
        x = x + gt2[:, None, :] * f
    return rmsnorm(x, g_final)
```

```python
import numpy as np
import ml_dtypes
import concourse.bass as bass
import concourse.mybir as mybir
from concourse.bass_utils import run_bass_kernel_spmd
from contextlib import ExitStack

F32 = mybir.dt.float32
BF16 = mybir.dt.bfloat16
AF = mybir.ActivationFunctionType
ALU = mybir.AluOpType
AX = mybir.AxisListType

T = 2048
D = 2048
NL = 4
DFF = 5632
NJ = 44
NE = 8
EPS = 1e-6
BIG = 30000.0
ENGS = ("pe", "act", "dve", "pool", "sp")


class Buf:
    __slots__ = ("name", "lw", "rd", "sem", "dcount", "excl")

    def __init__(self, name, excl=False):
        self.name = name
        self.excl = excl
        self.lw = None
        self.rd = []
        self.sem = None
        self.dcount = 0


class TK:
    def __init__(self, nc, es):
        self.nc = nc
        self.es = es
        self.eobj = {"pe": nc.tensor, "act": nc.scalar, "dve": nc.vector, "pool": nc.gpsimd, "sp": nc.sync}
        self.esem = {}
        self.ecount = {e: 0 for e in ENGS}
        self.known = {e: {} for e in ENGS}
        self.sems = {}
        self.dsems = {}
        self.nsem = 0
        self.ninst = {e: 0 for e in ENGS}
        for e in ENGS:
            self.esem[e] = self.new_sem("e_" + e)

    def new_sem(self, name):
        s = self.es.enter_context(self.nc.semaphore(name))
        self.nsem += 1
        key = "s%d" % self.nsem
        self.sems[key] = s
        return key

    def _need(self, eng, t, waits):
        if t is None:
            return
        key, val, src = t
        if self.known[eng].get(key, 0) >= val:
            return
        if waits.get(key, 0) < val:
            waits[key] = val

    def _emit_waits(self, eng, waits):
        for key, val in waits.items():
            self.eobj[eng].wait_ge(self.sems[key], val)
            self.known[eng][key] = val
            self.ninst[eng] += 1

    def op(self, eng, fn, reads=(), writes=(), signal=True):
        writes = list(writes) + [b for b in reads if b.excl]
        reads = [b for b in reads if not b.excl]
        waits = {}
        for b in reads:
            self._need(eng, b.lw, waits)
        for b in writes:
            if b.lw is not None and b.lw[2] != eng:
                self._need(eng, b.lw, waits)
            for t in b.rd:
                if t[2] != eng:
                    self._need(eng, t, waits)
        self._emit_waits(eng, waits)
        key = self.esem[eng]
        val = self.ecount[eng] + 1
        t = (key, val, eng)
        for b in reads:
            b.rd.append(t)
        for b in writes:
            b.lw = t
            b.rd = []
        ins = fn(self.eobj[eng])
        self.ninst[eng] += 1
        if signal:
            self.ecount[eng] = val
            ins.then_inc(self.sems[key], 1)

    def dma(self, eng, fn, sb, reads=(), writes=()):
        if sb.sem is None:
            sb.sem = self.new_sem("d_" + sb.name)
            self.dsems[sb.sem] = sb
        waits = {}
        for b in reads:
            self._need(eng, b.lw, waits)
        for b in writes:
            self._need(eng, b.lw, waits)
            for t in b.rd:
                self._need(eng, t, waits)
        self._emit_waits(eng, waits)
        sb.dcount += 16
        t = (sb.sem, sb.dcount, None)
        for b in reads:
            b.rd.append(t)
        for b in writes:
            b.lw = t
            b.rd = []
        fn(self.eobj[eng]).then_inc(self.sems[sb.sem], 16)
        self.ninst[eng] += 1

    def barrier(self, engs=ENGS):
        for e in engs:
            waits = {}
            for p in ENGS:
                if self.ecount[p] > 0:
                    self._need(e, (self.esem[p], self.ecount[p], p), waits)
            for key, sb in self.dsems.items():
                if sb.dcount > 0:
                    self._need(e, (key, sb.dcount, None), waits)
            self._emit_waits(e, waits)


def build_program(nl=NL, do_mix=True, do_ffn=True, dbg=False, moe_experts=NE, mixers=("conv", "ml", "att"),
                  nlw=NL, ndw=2, nmw=2):
    nc = bass.Bass("TRN2", target_bir_lowering=False)

    def din(name, shape, dt=F32):
        return nc.dram_tensor(name, list(shape), dt, kind="ExternalInput").ap()

    xT_in = din("xT", [D, T])
    condT = din("condT", [128, 16])
    wmod = din("wmod", [nlw, 24, 128, 16 * 512])
    bmodT = din("bmodT", [128, NL * 96])
    gmixT = din("gmixT", [128, NL * 16])
    gffnT = din("gffnT", [128, NL * 16])
    gfinT = din("gfinT", [128, 16])
    winF = din("winF", [nlw, 32, 128, 2048])
    winT = din("winT", [nlw, 16, 128, 2048])
    winG = din("winG", [NL, 128, 128])
    convw = din("convw", [128, NL * 4 * 31])
    convp = din("convp", [128, NL * 4 * 3])
    mlcw = din("mlcw", [128, NL * 8 * 4])
    mlcb = din("mlcb", [128, NL * 8])
    mlbif = din("mlbif", [128, NL * 128])
    mlng = din("mlng", [128, NL * 4])
    atng = din("atng", [128, NL * 8])
    wout = din("wout", [nlw, 16, 128, 2048])
    ffg = din("ffg", [ndw, NJ, 128, 2048])
    ffu = din("ffu", [ndw, NJ, 128, 2048])
    ffd = din("ffd", [ndw, 16, 128, NJ * 128])
    mog = din("mog", [nmw, NE, NJ, 128, 2048] if nmw else [1, 1, 1, 128, 2048])
    mou = din("mou", [nmw, NE, NJ, 128, 2048] if nmw else [1, 1, 1, 128, 2048])
    mod_ = din("mod", [nmw, NE, 16, 128, NJ * 128] if nmw else [1, 1, 1, 128, NJ * 128])
    rw = din("rw", [128, 2 * 16 * 8])
    rb = din("rb", [128, 2 * 32])
    c_identb = din("c_identb", [128, 128], BF16)
    c_ones = din("c_ones", [128, 128])
    c_tri = din("c_tri", [128, 128])
    c_trib = din("c_trib", [128, 128], BF16)
    c_esel = din("c_esel", [10, 8 * 128], BF16)
    c_e8 = din("c_e8", [8, 8 * 128], BF16)
    c_alibi = din("c_alibi", [128, 8 * 16 * 2], BF16)
    c_kpb = din("c_kpb", [128, 8 * 16])
    c_pastbias = din("c_pastbias", [128, 128])
    c_past01 = din("c_past01", [128, 128])
    c_own01 = din("c_own01", [128, 128])

    okind = "ExternalOutput"
    outT = nc.dram_tensor("outT", [D, T], F32, kind=okind).ap()
    skind = "ExternalOutput" if dbg else "Internal"
    XT = nc.dram_tensor("XT", [D, T], F32, kind=skind).ap()
    YT = nc.dram_tensor("YT", [D, T], BF16, kind=skind).ap()

    with ExitStack() as es:
        tk = TK(nc, es)
        bufs = {}

        def B(name, excl=False):
            if name not in bufs:
                bufs[name] = Buf(name, excl)
            return bufs[name]

        uid = [0]

        def sb(st, name, cols, dt=F32, parts=128):
            uid[0] += 1
            return st.enter_context(nc.sbuf_tensor("%s_%d" % (name, uid[0]), [parts, cols], dt))

        def V(fn, r, w, sig=True):
            tk.op("dve", fn, r, w, sig)

        def A(fn, r, w, sig=True):
            tk.op("act", fn, r, w, sig)

        def G(fn, r, w, sig=True):
            tk.op("pool", fn, r, w, sig)

        def P(fn, r, w, sig=True):
            tk.op("pe", fn, r, w, sig)

        def mm(out, lhsT, rhs, start, stop, r, w, sig=None, skip=False):
            if skip:
                P(lambda e: e.matmul(out, lhsT, rhs, start=start, stop=stop, skip_group_check=True), r, w,
                  stop if sig is None else sig)
            else:
                P(lambda e: e.matmul(out, lhsT, rhs, start=start, stop=stop), r, w, stop if sig is None else sig)

        NPS = 6
        ps = [es.enter_context(nc.psum_tensor("ps%d" % i, [128, 512], F32)) for i in range(NPS)]
        bps = [B("ps%d" % i, True) for i in range(NPS)]
        psb = es.enter_context(nc.psum_tensor("psb", [128, 2048], BF16))
        bpsb = [B("psb%d" % (i // 2), True) for i in range(4)]
        wk = [0]

        def nextps(n=4):
            i = wk[0] % n
            wk[0] += 1
            return i

        def load_const(name, src, cols, dt=F32, parts=128):
            t = sb(es, name, cols, dt, parts)
            b = B(name)
            tk.dma("sp", lambda e: e.dma_start(out=t[:], in_=src), b, writes=[b])
            return t, b

        identb, b_identb = load_const("identb", c_identb, 128, BF16)
        onesf, b_onesf = load_const("onesf", c_ones, 128)
        trif, b_trif = load_const("trif", c_tri, 128)
        trib, b_trib = load_const("tribb", c_trib, 128, BF16)
        esel, b_esel = load_const("esel", c_esel, 8 * 128, BF16, parts=10)
        e8, b_e8 = load_const("e8", c_e8, 8 * 128, BF16, parts=8)
        alibi, b_alibi = load_const("alibi", c_alibi, 8 * 16 * 2, BF16)
        kpb, b_kpb = load_const("kpb", c_kpb, 128)
        pastbias, b_pastbias = load_const("pastbias", c_pastbias, 128)
        past01, b_past01 = load_const("past01", c_past01, 128)
        own01, b_own01 = load_const("own01", c_own01, 128)
        bmod_s, b_bmod = load_const("bmods", bmodT, NL * 96)
        gmix_s, b_gmix = load_const("gmixs", gmixT, NL * 16)
        gffn_s, b_gffn = load_const("gffns", gffnT, NL * 16)
        gfin_s, b_gfin = load_const("gfins", gfinT, 16)
        convw_s, b_convw = load_const("convws", convw, NL * 4 * 31)
        convp_s, b_convp = load_const("convps", convp, NL * 4 * 3)
        mlcw_s, b_mlcw = load_const("mlcws", mlcw, NL * 8 * 4)
        mlcb_s, b_mlcb = load_const("mlcbs", mlcb, NL * 8)
        mlbif_s, b_mlbif = load_const("mlbifs", mlbif, NL * 128)
        mlng_s, b_mlng = load_const("mlngs", mlng, NL * 4)
        atng_s, b_atng = load_const("atngs", atng, NL * 8)
        rw_s, b_rw = load_const("rws", rw, 2 * 16 * 8)
        rb_s, b_rb = load_const("rbs", rb, 2 * 32)
        meanf = sb(es, "meanf", 128)
        b_meanf = B("meanf")
        V(lambda e: e.tensor_scalar(out=meanf[:], in0=onesf[:], scalar1=1.0 / 128.0, scalar2=None, op0=ALU.mult),
          [b_onesf], [b_meanf])
        epsc = sb(es, "epsc", 1)
        b_epsc = B("epsc")
        V(lambda e: e.memset(epsc[:], EPS), [], [b_epsc])
        onec = sb(es, "onec", 1)
        b_onec = B("onec")
        V(lambda e: e.memset(onec[:], 1.0), [], [b_onec])

        modT = sb(es, "modT", NL * 96)
        b_modT = B("modT")
        A1 = sb(es, "A1", NL * 16)
        A2 = sb(es, "A2", NL * 16)
        b_A = B("A12")

        with ExitStack() as st:
            cT = sb(st, "cT", 16)
            b_cT = B("cT")
            tk.dma("sp", lambda e: e.dma_start(out=cT[:], in_=condT), b_cT, writes=[b_cT])
            cS = sb(st, "cS", 16)
            b_cS = B("cS")
            A(lambda e: e.activation(out=cS[:], in_=cT[:], func=AF.Silu), [b_cT], [b_cS])
            wm = [sb(st, "wm%d" % i, 16 * 512) for i in range(2)]
            b_wm = [B("wm0"), B("wm1")]
            rowb = [sb(st, "rowb%d" % i, 512, F32, parts=1) for i in range(2)]
            b_rowb = [B("rowb0"), B("rowb1")]
            for l in range(nl):
                pi = 5
                for cb in range(24):
                    w_ = cb % 2
                    tk.dma("sp", lambda e, l=l, cb=cb, w_=w_: e.dma_start(out=wm[w_][:], in_=wmod[l, cb]),
                           b_wm[w_], writes=[b_wm[w_]])
                    pr = nextps()
                    for k in range(16):
                        mm(ps[pr][0:1, :], cS[:, k:k + 1], wm[w_][:, k * 512:(k + 1) * 512], k == 0, k == 15,
                           [b_wm[w_], b_cS], [bps[pr]])
                    A(lambda e, w_=w_, pr=pr: e.activation(out=rowb[w_][:], in_=ps[pr][0:1, :], func=AF.Copy),
                      [bps[pr]], [b_rowb[w_]])
                    for c4 in range(4):
                        j = cb * 4 + c4
                        mm(ps[pi][:, j:j + 1], rowb[w_][0:1, c4 * 128:(c4 + 1) * 128], onec[0:1, 0:1], True, True,
                           [b_rowb[w_], b_onec], [bps[pi]], sig=(c4 == 3))
                V(lambda e, l=l, pi=pi: e.tensor_tensor(out=modT[:, l * 96:(l + 1) * 96], in0=ps[pi][:, 0:96],
                                                        in1=bmod_s[:, l * 96:(l + 1) * 96], op=ALU.add),
                  [bps[pi], b_bmod], [b_modT])
                V(lambda e, l=l: e.scalar_tensor_tensor(out=A1[:, l * 16:(l + 1) * 16],
                                                        in0=modT[:, l * 96 + 16:l * 96 + 32], scalar=1.0,
                                                        in1=gmix_s[:, l * 16:(l + 1) * 16], op0=ALU.add, op1=ALU.mult),
                  [b_modT, b_gmix], [b_A])
                V(lambda e, l=l: e.scalar_tensor_tensor(out=A2[:, l * 16:(l + 1) * 16],
                                                        in0=modT[:, l * 96 + 64:l * 96 + 80], scalar=1.0,
                                                        in1=gffn_s[:, l * 16:(l + 1) * 16], op0=ALU.add, op1=ALU.mult),
                  [b_modT, b_gffn], [b_A])
            tk.barrier()

        def mcol(l, which, k):
            c = l * 96 + which * 16 + k
            return modT[:, c:c + 1]

        b_XT = [B("XT%d" % k) for k in range(16)]
        b_YT = [B("YT%d" % k) for k in range(16)]

        HTh = [None]
        b_HT = [B("HT%d" % k) for k in range(16)]

        def ht(k, a, b):
            return HTh[0][:, k * T + a:k * T + b]

        def phase_norm1(l, src):
            with ExitStack() as st:
                xk = [sb(st, "xk%d" % i, T) for i in range(2)]
                b_xk = [B("xk0"), B("xk1")]
                sq = [sb(st, "sq%d" % i, T) for i in range(2)]
                b_sq = [B("sq0"), B("sq1")]
                rsB = sb(st, "rsB", T)
                b_rsB = B("rsB")
                tmp = [sb(st, "tmp%d" % i, T) for i in range(2)]
                b_tmp = [B("tmp0"), B("tmp1")]
                for k in range(16):
                    i = k % 2
                    tk.dma("sp", lambda e, k=k, i=i: e.dma_start(out=xk[i][:], in_=src[k * 128:(k + 1) * 128, :]),
                           b_xk[i], reads=[b_XT[k]], writes=[b_xk[i]])
                    A(lambda e, i=i: e.activation(out=sq[i][:], in_=xk[i][:], func=AF.Square), [b_xk[i]], [b_sq[i]])
                    for t in range(4):
                        mm(ps[t][:], onesf[:], sq[i][:, t * 512:(t + 1) * 512], k == 0, k == 15,
                           [b_onesf, b_sq[i]], [bps[t]], sig=(k == 15 or t == 3))
                for t in range(4):
                    A(lambda e, t=t: e.activation(out=rsB[:, t * 512:(t + 1) * 512], in_=ps[t][:], func=AF.Sqrt,
                                                  bias=epsc[:], scale=1.0 / D), [bps[t], b_epsc], [b_rsB])
                V(lambda e: e.reciprocal(out=rsB[:], in_=rsB[:]), [b_rsB], [b_rsB])
                for k in range(16):
                    i = k % 2
                    tk.dma("sp", lambda e, k=k, i=i: e.dma_start(out=xk[i][:], in_=src[k * 128:(k + 1) * 128, :]),
                           b_xk[i], reads=[b_XT[k]], writes=[b_xk[i]])
                    V(lambda e, i=i: e.tensor_tensor(out=tmp[i][:], in0=xk[i][:], in1=rsB[:], op=ALU.mult),
                      [b_xk[i], b_rsB], [b_tmp[i]])
                    A(lambda e, k=k, i=i: e.activation(out=ht(k, 0, T), in_=tmp[i][:], func=AF.Identity,
                                                       bias=mcol(l, 0, k), scale=A1[:, l * 16 + k:l * 16 + k + 1]),
                      [b_tmp[i], b_modT, b_A], [b_HT[k]])
                tk.barrier()

        def proj_fm(wb, b_wb, t, pi):
            for k in range(16):
                mm(ps[pi][:], wb[:, k * 128:(k + 1) * 128], ht(k, t * 512, (t + 1) * 512), k == 0, k == 15,
                   [b_wb, b_HT[k]], [bps[pi]])

        def proj_tm(wb, b_wb, tt, out_ap, b_out, ncols=128):
            for k in range(16):
                mm(out_ap, ht(k, tt * 128, (tt + 1) * 128), wb[:, k * ncols:(k + 1) * ncols], k == 0, k == 15,
                   [b_wb, b_HT[k]], [b_out])

        def phase_mixer(l):
            with ExitStack() as st:
                wbs = [sb(st, "wb%d" % i, 2048, BF16) for i in range(3)]
                b_wbs = [B("wb%d" % i) for i in range(3)]
                wi = [0]

                def loadw(src):
                    i = wi[0] % 3
                    wi[0] += 1
                    tk.dma("pool", lambda e: e.dma_start(out=wbs[i][:], in_=src), b_wbs[i], writes=[b_wbs[i]])
                    return wbs[i], b_wbs[i]

                yT = [sb(st, "yT%d" % i, T, BF16) for i in range(2)]
                b_yT = [B("yT0"), B("yT1")]
                yi = [0]

                def store_y(row, i):
                    tk.dma("sp", lambda e: e.dma_start(out=YT[row * 128:(row + 1) * 128, :], in_=yT[i][:]),
                           b_yT[i], reads=[b_yT[i]], writes=[b_YT[row]])

                if "conv" in mixers:
                    with ExitStack() as s2:
                        upad = sb(s2, "upad", 30 + T, BF16)
                        b_upad = B("upad")
                        dg = [sb(s2, "dg%d" % i, 31 * 128, BF16) for i in range(2)]
                        b_dg = [B("dg0"), B("dg1")]
                        acc = sb(s2, "acc", T)
                        b_acc = B("acc")
                        cen = sb(s2, "cen", T)
                        b_cen = B("cen")
                        sq = sb(s2, "csq", T)
                        b_sq = B("csq")
                        sgt = [sb(s2, "sgt%d" % i, 512) for i in range(2)]
                        b_sgt = [B("sgt0"), B("sgt1")]
                        rst = [sb(s2, "rst%d" % i, 512) for i in range(2)]
                        b_rst = [B("rst0"), B("rst1")]
                        V(lambda e: e.memset(upad[:, 0:30], 0.0), [], [b_upad])
                        for j in range(4):
                            wa, b_wa = loadw(winF[l, j])
                            wg_, b_wg = loadw(winF[l, 4 + j])
                            for t in range(4):
                                pa = nextps()
                                proj_fm(wa, b_wa, t, pa)
                                pg = nextps()
                                proj_fm(wg_, b_wg, t, pg)
                                i = t % 2
                                A(lambda e, i=i, pg=pg: e.activation(out=sgt[i][:], in_=ps[pg][:], func=AF.Sigmoid),
                                  [bps[pg]], [b_sgt[i]])
                                V(lambda e, i=i, pa=pa, t=t: e.tensor_tensor(
                                    out=upad[:, 30 + t * 512:30 + (t + 1) * 512], in0=ps[pa][:], in1=sgt[i][:],
                                    op=ALU.mult), [bps[pa], b_sgt[i]], [b_upad])
                            cw0 = (l * 4 + j) * 31
                            cp0 = (l * 4 + j) * 3
                            di = j % 2
                            for jj in range(31):
                                V(lambda e, jj=jj, cw0=cw0, di=di: e.tensor_scalar(
                                    out=dg[di][:, jj * 128:(jj + 1) * 128], in0=identb[:],
                                    scalar1=convw_s[:, cw0 + jj:cw0 + jj + 1], scalar2=None, op0=ALU.mult),
                                  [b_identb, b_convw], [b_dg[di]], sig=(jj == 30))
                            for t in range(4):
                                pc = nextps()
                                for jj in range(31):
                                    mm(ps[pc][:], dg[di][:, jj * 128:(jj + 1) * 128],
                                       upad[:, jj + t * 512:jj + (t + 1) * 512], jj == 0, jj == 30,
                                       [b_dg[di], b_upad], [bps[pc]])
                                A(lambda e, pc=pc, t=t, cp0=cp0: e.activation(
                                    out=acc[:, t * 512:(t + 1) * 512], in_=ps[pc][:], func=AF.Identity,
                                    bias=convp_s[:, cp0:cp0 + 1], scale=1.0), [bps[pc], b_convp], [b_acc])
                            y_i = yi[0] % 2
                            yi[0] += 1
                            for t in range(4):
                                sl = slice(t * 512, (t + 1) * 512)
                                pm = nextps()
                                mm(ps[pm][:], meanf[:], acc[:, sl], True, True, [b_meanf, b_acc], [bps[pm]])
                                V(lambda e, sl=sl, pm=pm: e.tensor_tensor(out=cen[:, sl], in0=acc[:, sl],
                                                                          in1=ps[pm][:], op=ALU.subtract),
                                  [b_acc, bps[pm]], [b_cen])
                                A(lambda e, sl=sl: e.activation(out=sq[:, sl], in_=cen[:, sl], func=AF.Square),
                                  [b_cen], [b_sq])
                                pv = nextps()
                                mm(ps[pv][:], meanf[:], sq[:, sl], True, True, [b_meanf, b_sq], [bps[pv]])
                                i = t % 2
                                A(lambda e, i=i, pv=pv: e.activation(out=rst[i][:], in_=ps[pv][:], func=AF.Sqrt,
                                                                     bias=epsc[:], scale=1.0),
                                  [bps[pv], b_epsc], [b_rst[i]])
                                V(lambda e, i=i: e.reciprocal(out=rst[i][:], in_=rst[i][:]), [b_rst[i]], [b_rst[i]])
                                V(lambda e, i=i, sl=sl: e.tensor_tensor(out=cen[:, sl], in0=cen[:, sl], in1=rst[i][:],
                                                                        op=ALU.mult), [b_cen, b_rst[i]], [b_cen])
                                A(lambda e, sl=sl, y_i=y_i, cp0=cp0: e.activation(
                                    out=yT[y_i][:, sl], in_=cen[:, sl], func=AF.Silu,
                                    bias=convp_s[:, cp0 + 2:cp0 + 3], scale=convp_s[:, cp0 + 1:cp0 + 2]),
                                  [b_cen, b_convp], [b_yT[y_i]])
                            store_y(j, y_i)
                        tk.barrier()

                if "ml" in mixers:
                    with ExitStack() as s2:
                        gpre = sb(s2, "gpre", 128)
                        b_gpre = B("gpre")
                        GI = sb(s2, "GI", 64)
                        EZ = sb(s2, "EZ", 64)
                        LF = sb(s2, "LF", 64)
                        AA = sb(s2, "AA", 64)
                        EB = sb(s2, "EB", 64)
                        EBL = sb(s2, "EBL", 64)
                        b_g = B("gates")
                        b_g2 = B("gates2")
                        wg_ = sb(s2, "wgates", 128, BF16)
                        b_wg = B("wgates")
                        tk.dma("pool", lambda e: e.dma_start(out=wg_[:], in_=winG[l]), b_wg, writes=[b_wg])
                        pg = 4
                        for tt in range(16):
                            proj_tm(wg_, b_wg, tt, ps[pg][:, tt * 8:(tt + 1) * 8], bps[pg], ncols=8)
                        V(lambda e: e.tensor_tensor(out=gpre[:], in0=ps[pg][:, 0:128],
                                                    in1=mlbif_s[:, l * 128:(l + 1) * 128], op=ALU.add),
                          [bps[pg], b_mlbif], [b_gpre])
                        g3 = gpre[:].rearrange("p (t e) -> p t e", e=8)

                        def v3(tl):
                            return tl[:].rearrange("p (t e) -> p t e", e=4)

                        V(lambda e: e.tensor_copy(out=v3(GI), in_=g3[:, :, 0:4]), [b_gpre], [b_g])
                        A(lambda e: e.activation(out=v3(EZ), in_=g3[:, :, 4:8], func=AF.Exp, scale=-1.0),
                          [b_gpre], [b_g])
                        A(lambda e: e.activation(out=LF[:], in_=EZ[:], func=AF.Ln, bias=onec[:], scale=1.0),
                          [b_g, b_onec], [b_g])
                        pB, pL = 4, 5
                        mm(ps[pB][:, 0:64], trif[:], LF[:], True, True, [b_trif, b_g], [bps[pB]])
                        mm(ps[pL][:, 0:64], onesf[:], LF[:], True, True, [b_onesf, b_g], [bps[pL]])
                        V(lambda e: e.tensor_tensor(out=AA[:], in0=GI[:], in1=ps[pB][:, 0:64], op=ALU.add),
                          [b_g, bps[pB]], [b_g2])
                        A(lambda e: e.activation(out=AA[:], in_=AA[:], func=AF.Exp), [b_g2], [b_g2])
                        A(lambda e: e.activation(out=EB[:], in_=ps[pB][:, 0:64], func=AF.Exp, scale=-1.0),
                          [bps[pB]], [b_g2])
                        A(lambda e: e.activation(out=EBL[:], in_=ps[pL][:, 0:64], func=AF.Exp, scale=-1.0),
                          [bps[pL]], [b_g2])

                        qpad = sb(s2, "qpad", 3 + T)
                        b_qpad = B("qpad")
                        cacc = sb(s2, "cacc", T)
                        b_cacc = B("cacc")
                        qT = sb(s2, "qT", T, BF16)
                        b_qT = B("qT")
                        kT = sb(s2, "kT", T, BF16)
                        b_kT = B("kT")
                        ktm = sb(s2, "ktm", T, BF16)
                        b_ktm = B("ktm")
                        va = sb(s2, "va", 16 * 129, BF16)
                        b_va = B("va")
                        og = sb(s2, "og", T, BF16)
                        b_og = B("og")
                        PT = [sb(s2, "mPT%d" % i, 128, BF16) for i in range(2)]
                        b_PT = [B("mPT0"), B("mPT1")]
                        Cf = sb(s2, "Cf", 129)
                        b_Cf = B("Cf")
                        Ct = sb(s2, "Ct", 129)
                        b_Ct = B("Ct")
                        Cb = [sb(s2, "Cb%d" % i, 129, BF16) for i in range(2)]
                        b_Cb = [B("Cb0"), B("Cb1")]
                        smM = [sb(s2, "sm%d" % i, 8) for i in range(2)]
                        b_smM = [B("sm0"), B("sm1")]
                        hr = [sb(s2, "hr%d" % i, 128) for i in range(2)]
                        b_hr = [B("hr0"), B("hr1")]
                        junkM = [sb(s2, "junk%d" % i, 128) for i in range(2)]
                        b_junkM = [B("junk0"), B("junk1")]
                        hn = [sb(s2, "hn%d" % i, 128, BF16) for i in range(2)]
                        b_hn = [B("hn0"), B("hn1")]
                        V(lambda e: e.memset(qpad[:, 0:3], 0.0), [], [b_qpad])
                        dhs = 128.0 ** -0.5

                        for hd in range(4):
                            def qk_path(chunk, dst, b_dst, scale_k):
                                wq, b_wq = loadw(winF[l, chunk])
                                for t in range(4):
                                    pi = nextps()
                                    proj_fm(wq, b_wq, t, pi)
                                    A(lambda e, pi=pi, t=t: e.activation(out=qpad[:, 3 + t * 512:3 + (t + 1) * 512],
                                                                         in_=ps[pi][:], func=AF.Copy),
                                      [bps[pi]], [b_qpad])
                                ch = chunk - 8
                                w0 = (l * 8 + ch) * 4
                                V(lambda e: e.tensor_scalar(out=cacc[:], in0=qpad[:, 0:T], scalar1=mlcw_s[:, w0:w0 + 1],
                                                            scalar2=mlcb_s[:, l * 8 + ch:l * 8 + ch + 1],
                                                            op0=ALU.mult, op1=ALU.add),
                                  [b_qpad, b_mlcw, b_mlcb], [b_cacc])
                                for jj in range(1, 4):
                                    V(lambda e, jj=jj: e.scalar_tensor_tensor(
                                        out=cacc[:], in0=qpad[:, jj:jj + T], scalar=mlcw_s[:, w0 + jj:w0 + jj + 1],
                                        in1=cacc[:], op0=ALU.mult, op1=ALU.add), [b_qpad, b_mlcw, b_cacc], [b_cacc])
                                if not scale_k:
                                    A(lambda e: e.activation(out=dst[:], in_=cacc[:], func=AF.Silu), [b_cacc], [b_dst])
                                else:
                                    A(lambda e: e.activation(out=cacc[:], in_=cacc[:], func=AF.Silu),
                                      [b_cacc], [b_cacc])
                                    V(lambda e: e.tensor_scalar(out=dst[:], in0=cacc[:], scalar1=dhs, scalar2=None,
                                                                op0=ALU.mult), [b_cacc], [b_dst])

                            qk_path(8 + hd, qT, b_qT, False)
                            qk_path(12 + hd, kT, b_kT, True)
                            for c in range(16):
                                qd = c // 4
                                P(lambda e, c=c: e.transpose(psb[:, c * 128:(c + 1) * 128], kT[:, c * 128:(c + 1) * 128],
                                                             identb[:]), [b_kT, b_identb], [bpsb[qd]], sig=(c % 4 == 3))
                                if c % 4 == 3:
                                    V(lambda e, qd=qd: e.tensor_copy(out=ktm[:, qd * 512:(qd + 1) * 512],
                                                                     in_=psb[:, qd * 512:(qd + 1) * 512]),
                                      [bpsb[qd]], [b_ktm])
                            wv, b_wv = loadw(winT[l, hd])
                            for tt in range(16):
                                pi = nextps()
                                proj_tm(wv, b_wv, tt, ps[pi][:, 0:128], bps[pi])
                                col = tt * 4 + hd
                                V(lambda e, pi=pi, tt=tt, col=col: e.tensor_scalar(
                                    out=va[:, tt * 129:tt * 129 + 128], in0=ps[pi][:, 0:128],
                                    scalar1=AA[:, col:col + 1], scalar2=None, op0=ALU.mult),
                                  [bps[pi], b_g2], [b_va])
                                V(lambda e, tt=tt, col=col: e.tensor_copy(out=va[:, tt * 129 + 128:tt * 129 + 129],
                                                                          in_=AA[:, col:col + 1]), [b_g2], [b_va])
                            wo, b_wo = loadw(winT[l, 4 + hd])
                            for tt in range(16):
                                pi = nextps()
                                proj_tm(wo, b_wo, tt, ps[pi][:, 0:128], bps[pi])
                                A(lambda e, pi=pi, tt=tt: e.activation(out=og[:, tt * 128:(tt + 1) * 128],
                                                                       in_=ps[pi][:, 0:128], func=AF.Sigmoid),
                                  [bps[pi]], [b_og])
                            y_i = yi[0] % 2
                            yi[0] += 1
                            def emitSm(c):
                                cs_ = slice(c * 128, (c + 1) * 128)
                                pS_ = nextps()
                                mm(ps[pS_][:, 0:128], kT[:, cs_], qT[:, cs_], True, True, [b_kT, b_qT], [bps[pS_]])
                                return pS_

                            mpend = []
                            nxtSm = emitSm(0)
                            for c in range(16):
                                cs = slice(c * 128, (c + 1) * 128)
                                col = c * 4 + hd
                                pS = nxtSm
                                if c < 15:
                                    nxtSm = emitSm(c + 1)
                                pt = c % 2
                                sm = smM[c % 2]
                                b_sm = b_smM[c % 2]
                                junk = junkM[c % 2]
                                b_junk = b_junkM[c % 2]
                                V(lambda e, pS=pS, pt=pt: e.tensor_tensor(out=PT[pt][:], in0=ps[pS][:, 0:128],
                                                                          in1=trib[:], op=ALU.mult),
                                  [bps[pS], b_trib], [b_PT[pt]])
                                pO = nextps()
                                mm(ps[pO][:, 0:129], PT[pt][:], va[:, c * 129:(c + 1) * 129], True, c == 0,
                                   [b_PT[pt], b_va], [bps[pO]])
                                if c > 0:
                                    cbp = (c - 1) % 2
                                    mm(ps[pO][:, 0:129], qT[:, cs], Cb[cbp][:], False, True, [b_qT, b_Cb[cbp]],
                                       [bps[pO]])
                                if c < 15:
                                    pU = nextps()
                                    mm(ps[pU][:, 0:129], ktm[:, cs], va[:, c * 129:(c + 1) * 129], True, True,
                                       [b_ktm, b_va], [bps[pU]])
                                    if c == 0:
                                        V(lambda e, pU=pU: e.tensor_copy(out=Ct[:], in_=ps[pU][:, 0:129]),
                                          [bps[pU]], [b_Ct])
                                    else:
                                        V(lambda e, pU=pU: e.tensor_tensor(out=Ct[:], in0=ps[pU][:, 0:129], in1=Cf[:],
                                                                           op=ALU.add), [bps[pU], b_Cf], [b_Ct])
                                    V(lambda e, col=col: e.tensor_scalar(out=Cf[:], in0=Ct[:],
                                                                         scalar1=EBL[:, col:col + 1], scalar2=None,
                                                                         op0=ALU.mult), [b_Ct, b_g2], [b_Cf])
                                    cbn = c % 2
                                    V(lambda e, cbn=cbn: e.tensor_copy(out=Cb[cbn][:], in_=Cf[:]), [b_Cf], [b_Cb[cbn]])
                                hi = c % 2
                                A(lambda e, pO=pO, col=col: e.activation(out=sm[:, 0:1], in_=ps[pO][:, 128:129],
                                                                         func=AF.Abs, scale=EB[:, col:col + 1]),
                                  [bps[pO], b_g2], [b_sm])
                                V(lambda e: e.tensor_scalar(out=sm[:, 1:2], in0=sm[:, 0:1], scalar1=1.0, scalar2=None,
                                                            op0=ALU.max), [b_sm], [b_sm])
                                V(lambda e: e.reciprocal(out=sm[:, 6:7], in_=sm[:, 1:2]), [b_sm], [b_sm])
                                V(lambda e, col=col: e.tensor_tensor(out=sm[:, 2:3], in0=EB[:, col:col + 1],
                                                                     in1=sm[:, 6:7], op=ALU.mult),
                                  [b_sm, b_g2], [b_sm])
                                A(lambda e, pO=pO, hi=hi: e.activation(out=hr[hi][:], in_=ps[pO][:, 0:128],
                                                                       func=AF.Copy, scale=sm[:, 2:3]),
                                  [bps[pO], b_sm], [b_hr[hi]])
                                A(lambda e, hi=hi: e.activation(out=junk[:], in_=hr[hi][:], func=AF.Square,
                                                                accum_out=sm[:, 3:4]), [b_hr[hi]], [b_junk, b_sm])
                                A(lambda e: e.activation(out=sm[:, 4:5], in_=sm[:, 3:4], func=AF.Sqrt, bias=epsc[:],
                                                         scale=1.0 / 128.0), [b_sm, b_epsc], [b_sm])
                                V(lambda e: e.reciprocal(out=sm[:, 5:6], in_=sm[:, 4:5]), [b_sm], [b_sm])
                                V(lambda e, hi=hi, cs=cs: e.scalar_tensor_tensor(
                                    out=hn[hi][:], in0=hr[hi][:], scalar=sm[:, 5:6], in1=og[:, cs], op0=ALU.mult,
                                    op1=ALU.mult), [b_hr[hi], b_sm, b_og], [b_hn[hi]])
                                qd = c % 4

                                def mlate(hi=hi, qd=qd, cs=cs, y_i=y_i, hd=hd):
                                    P(lambda e: e.transpose(psb[:, qd * 512:qd * 512 + 128], hn[hi][:], identb[:]),
                                      [b_hn[hi], b_identb], [bpsb[qd]])
                                    A(lambda e: e.activation(
                                        out=yT[y_i][:, cs], in_=psb[:, qd * 512:qd * 512 + 128], func=AF.Copy,
                                        scale=mlng_s[:, l * 4 + hd:l * 4 + hd + 1]), [bpsb[qd], b_mlng], [b_yT[y_i]])
                                mpend.append(mlate)
                                if len(mpend) > 1:
                                    mpend.pop(0)()
                            while mpend:
                                mpend.pop(0)()
                            store_y(4 + hd, y_i)
                        tk.barrier()

                if "att" in mixers:
                    with ExitStack() as s2:
                        qT = sb(s2, "aqT", T, BF16)
                        b_qT = B("aqT")
                        kT = sb(s2, "akT", T, BF16)
                        b_kT = B("akT")
                        vx = sb(s2, "vx", 16 * 129, BF16)
                        b_vx = B("vx")
                        kmr = sb(s2, "kmr", 8)
                        b_kmr = B("kmr")
                        kmb = sb(s2, "kmb", 8, BF16)
                        b_kmb = B("kmb")
                        gm = sb(s2, "gm", 128)
                        b_gm = B("gm")
                        m8 = sb(s2, "m8", 128)
                        b_m8 = B("m8")
                        sel = sb(s2, "sel", 128)
                        b_sel = B("sel")
                        MBp = sb(s2, "MBp", 160, BF16)
                        b_MBp = B("MBp")
                        R = sb(s2, "R", T, BF16, parts=10)
                        b_R = B("R")
                        PT = [sb(s2, "aPT%d" % i, 512, BF16) for i in range(2)]
                        b_PT = [B("aPT0"), B("aPT1")]
                        smA = [sb(s2, "asm%d" % i, 8) for i in range(2)]
                        b_smA = [B("asm0"), B("asm1")]
                        o_ = [sb(s2, "ao%d" % i, 128) for i in range(2)]
                        b_o = [B("ao0"), B("ao1")]
                        junkA = [sb(s2, "ajunk%d" % i, 128) for i in range(2)]
                        b_junkA = [B("ajunk0"), B("ajunk1")]
                        pend = []
                        fpend = []
                        on = [sb(s2, "aon%d" % i, 128, BF16) for i in range(2)]
                        b_on = [B("aon0"), B("aon1")]
                        oreg = [(2, 0), (3, 0), (4, 0), (5, 0)]
                        b_oreg = [bps[2], bps[3], bps[4], bps[5]]
                        dhs = 128.0 ** -0.5
                        V(lambda e: e.memset(vx[:], 1.0), [], [b_vx])
                        fin = [0]
                        for h in range(8):
                            wq, b_wq = loadw(winF[l, 16 + h])
                            for t in range(4):
                                pi = nextps()
                                proj_fm(wq, b_wq, t, pi)
                                A(lambda e, pi=pi, t=t: e.activation(out=qT[:, t * 512:(t + 1) * 512], in_=ps[pi][:],
                                                                     func=AF.Copy, scale=dhs), [bps[pi]], [b_qT])
                            wk_, b_wk = loadw(winF[l, 24 + h])
                            for t in range(4):
                                pi = nextps()
                                proj_fm(wk_, b_wk, t, pi)
                                A(lambda e, pi=pi, t=t: e.activation(out=kT[:, t * 512:(t + 1) * 512], in_=ps[pi][:],
                                                                     func=AF.Copy), [bps[pi]], [b_kT])
                                V(lambda e, pi=pi, t=t: e.tensor_reduce(
                                    out=kmr[:, 2 * t:2 * t + 2], in_=ps[pi][:].rearrange("p (b s) -> p b s", s=256),
                                    axis=AX.X, op=ALU.add), [bps[pi]], [b_kmr])
                            V(lambda e: e.tensor_scalar(out=kmb[:], in0=kmr[:], scalar1=1.0 / 256.0, scalar2=None,
                                                        op0=ALU.mult), [b_kmr], [b_kmb])
                            wv, b_wv = loadw(winT[l, 8 + h])
                            for tt in range(16):
                                pi = nextps()
                                proj_tm(wv, b_wv, tt, ps[pi][:, 0:128], bps[pi])
                                V(lambda e, pi=pi, tt=tt: e.tensor_copy(out=vx[:, tt * 129:tt * 129 + 128],
                                                                        in_=ps[pi][:, 0:128]), [bps[pi]], [b_vx])
                            pg = nextps()
                            for tt in range(16):
                                mm(ps[pg][:, tt * 8:(tt + 1) * 8], qT[:, tt * 128:(tt + 1) * 128], kmb[:], True, True,
                                   [b_qT, b_kmb], [bps[pg]], sig=(tt == 15))
                            V(lambda e, pg=pg: e.tensor_tensor(out=gm[:], in0=ps[pg][:, 0:128], in1=pastbias[:],
                                                               op=ALU.add), [bps[pg], b_pastbias], [b_gm])
                            for tt in range(16):
                                V(lambda e, tt=tt: e.max(out=m8[:, tt * 8:(tt + 1) * 8], in_=gm[:, tt * 8:(tt + 1) * 8]),
                                  [b_gm], [b_m8], sig=(tt == 15))
                            for tt in range(16):
                                V(lambda e, tt=tt: e.tensor_scalar(out=sel[:, tt * 8:(tt + 1) * 8],
                                                                   in0=gm[:, tt * 8:(tt + 1) * 8],
                                                                   scalar1=m8[:, tt * 8 + 2:tt * 8 + 3], scalar2=None,
                                                                   op0=ALU.is_ge), [b_gm, b_m8], [b_sel],
                                  sig=(tt == 15))
                            V(lambda e: e.tensor_tensor(out=sel[:], in0=sel[:], in1=past01[:], op=ALU.mult),
                              [b_sel, b_past01], [b_sel])
                            V(lambda e: e.tensor_tensor(out=sel[:], in0=sel[:], in1=own01[:], op=ALU.add),
                              [b_sel, b_own01], [b_sel])
                            mb3 = MBp[:].rearrange("p (t c) -> p t c", c=10)
                            V(lambda e: e.tensor_scalar(out=mb3[:, :, 2:10],
                                                        in0=sel[:].rearrange("p (t c) -> p t c", c=8), scalar1=-1.0,
                                                        scalar2=BIG, op0=ALU.add, op1=ALU.mult), [b_sel], [b_MBp])
                            V(lambda e, h=h: e.tensor_copy(
                                out=mb3[:, :, 0:2],
                                in_=alibi[:, h * 32:(h + 1) * 32].rearrange("p (t c) -> p t c", c=2)),
                              [b_alibi], [b_MBp])
                            for tt in range(16):
                                qd = tt // 4
                                P(lambda e, tt=tt: e.transpose(psb[0:10, tt * 128:(tt + 1) * 128],
                                                               MBp[:, tt * 10:(tt + 1) * 10], identb[:]),
                                  [b_MBp, b_identb], [bpsb[qd]], sig=(tt % 4 == 3))
                            A(lambda e: e.activation(out=R[:], in_=psb[0:10, :], func=AF.Copy), bpsb, [b_R])
                            y_i = yi[0] % 2
                            yi[0] += 1
                            its = [(g, kt) for g in range(4) for kt in range(4 * g + 4)]

                            def emitS(g, kt):
                                off = max(0, kt - 4 * g) * 128
                                N = 512 - off
                                q0 = g * 512 + off
                                n = kt // 2
                                pS = nextps(2)
                                mm(ps[pS][:, 0:N], kT[:, kt * 128:(kt + 1) * 128], qT[:, q0:q0 + N], True, False,
                                   [b_kT, b_qT], [bps[pS]], sig=False)
                                mm(ps[pS][:, 0:N], esel[:, n * 128:(n + 1) * 128], R[:, q0:q0 + N], False, True,
                                   [b_esel, b_R], [bps[pS]])
                                return pS

                            nxtS = emitS(*its[0])
                            for idx, (g, kt) in enumerate(its):
                                if True:
                                    off = max(0, kt - 4 * g) * 128
                                    N = 512 - off
                                    pS = nxtS
                                    if idx + 1 < len(its):
                                        nxtS = emitS(*its[idx + 1])
                                    while pend:
                                        pend.pop(0)()
                                    if kt == 0:
                                        while fpend:
                                            fpend.pop(0)()
                                        for bk_ in (2, 3, 4, 5):
                                            V(lambda e, bk_=bk_: e.memset(ps[bk_][:, 0:129], 0.0), [], [bps[bk_]])
                                    pt = fin[0] % 2
                                    fin[0] += 1
                                    A(lambda e, pS=pS, N=N, pt=pt, kt=kt, h=h: e.activation(
                                        out=PT[pt][:, 0:N], in_=ps[pS][:, 0:N], func=AF.Exp,
                                        bias=kpb[:, h * 16 + kt:h * 16 + kt + 1], scale=1.0),
                                      [bps[pS], b_kpb], [b_PT[pt]])
                                    if kt >= 4 * g:
                                        V(lambda e, pt=pt: e.tensor_tensor(out=PT[pt][:, 0:128], in0=PT[pt][:, 0:128],
                                                                           in1=trib[:], op=ALU.mult),
                                          [b_PT[pt], b_trib], [b_PT[pt]])
                                    for i in range(max(4 * g, kt), 4 * g + 4):
                                        ii = i - 4 * g
                                        col = ii * 128 - off
                                        bk, c0 = oreg[ii]
                                        mm(ps[bk][:, c0:c0 + 129], PT[pt][:, col:col + 128],
                                           vx[:, kt * 129:(kt + 1) * 129], False, kt == i, [b_PT[pt], b_vx],
                                           [b_oreg[ii]], sig=True, skip=True)
                                    while fpend:
                                        fpend.pop(0)()
                                    if kt >= 4 * g:
                                        def finz(kt=kt, g=g, y_i=y_i, h=h):
                                            i = kt
                                            ii = i - 4 * g
                                            bk, c0 = oreg[ii]
                                            oi = i % 2
                                            sm = smA[oi]
                                            b_sm = b_smA[oi]
                                            junk = junkA[oi]
                                            b_junk = b_junkA[oi]
                                            cs = slice(i * 128, (i + 1) * 128)
                                            V(lambda e, bk=bk, c0=c0, sm=sm: e.reciprocal(
                                                out=sm[:, 0:1], in_=ps[bk][:, c0 + 128:c0 + 129]), [b_oreg[ii]], [b_sm])
                                            A(lambda e, bk=bk, c0=c0, oi=oi, sm=sm: e.activation(
                                                out=o_[oi][:], in_=ps[bk][:, c0:c0 + 128], func=AF.Copy, scale=sm[:, 0:1]),
                                              [b_oreg[ii], b_sm], [b_o[oi]])
                                            A(lambda e, oi=oi, sm=sm, junk=junk: e.activation(
                                                out=junk[:], in_=o_[oi][:], func=AF.Square, accum_out=sm[:, 1:2]),
                                              [b_o[oi]], [b_junk, b_sm])
                                            A(lambda e, sm=sm: e.activation(out=sm[:, 2:3], in_=sm[:, 1:2], func=AF.Ln,
                                                                            bias=epsc[:], scale=1.0 / 128.0),
                                              [b_sm, b_epsc], [b_sm])
                                            A(lambda e, sm=sm: e.activation(out=sm[:, 3:4], in_=sm[:, 2:3], func=AF.Exp,
                                                                            scale=-0.5), [b_sm], [b_sm])
                                            V(lambda e, oi=oi, sm=sm: e.tensor_scalar(out=on[oi][:], in0=o_[oi][:],
                                                                                      scalar1=sm[:, 3:4], scalar2=None,
                                                                                      op0=ALU.mult),
                                              [b_o[oi], b_sm], [b_on[oi]])
                                            qd = i % 4

                                            def late(oi=oi, qd=qd, cs=cs, y_i=y_i, h=h):
                                                P(lambda e: e.transpose(psb[:, qd * 512:qd * 512 + 128], on[oi][:],
                                                                        identb[:]), [b_on[oi], b_identb], [bpsb[qd]])
                                                A(lambda e: e.activation(
                                                    out=yT[y_i][:, cs], in_=psb[:, qd * 512:qd * 512 + 128], func=AF.Copy,
                                                    scale=atng_s[:, l * 8 + h:l * 8 + h + 1]), [bpsb[qd], b_atng],
                                                  [b_yT[y_i]])
                                            pend.append(late)
                                        fpend.append(finz)
                            while fpend:
                                fpend.pop(0)()
                            while pend:
                                pend.pop(0)()
                            store_y(8 + h, y_i)
                        tk.barrier()
                tk.barrier()

        def phase_outproj(l, src):
            with ExitStack() as st:
                wbs = [sb(st, "wo%d" % i, 2048, BF16) for i in range(2)]
                b_wbs = [B("wo0"), B("wo1")]
                xk = [sb(st, "oxk%d" % i, T) for i in range(2)]
                b_xk = [B("oxk0"), B("oxk1")]
                xo = [sb(st, "oxo%d" % i, T) for i in range(2)]
                b_xo = [B("oxo0"), B("oxo1")]
                for k in range(16):
                    tk.dma("sp", lambda e, k=k: e.dma_start(out=ht(k, 0, T), in_=YT[k * 128:(k + 1) * 128, :]),
                           b_HT[k], reads=[b_YT[k]], writes=[b_HT[k]])
                for m in range(16):
                    i = m % 2
                    tk.dma("pool", lambda e, m=m, i=i: e.dma_start(out=wbs[i][:], in_=wout[l, m]), b_wbs[i],
                           writes=[b_wbs[i]])
                    tk.dma("sp", lambda e, m=m, i=i: e.dma_start(out=xk[i][:], in_=src[m * 128:(m + 1) * 128, :]),
                           b_xk[i], reads=[b_XT[m]], writes=[b_xk[i]])
                    for t in range(4):
                        pi = nextps()
                        proj_fm(wbs[i], b_wbs[i], t, pi)
                        sl = slice(t * 512, (t + 1) * 512)
                        V(lambda e, pi=pi, sl=sl, i=i, m=m: e.scalar_tensor_tensor(
                            out=xo[i][:, sl], in0=ps[pi][:], scalar=mcol(l, 2, m), in1=xk[i][:, sl], op0=ALU.mult,
                            op1=ALU.add), [bps[pi], b_modT, b_xk[i]], [b_xo[i]])
                    tk.dma("sp", lambda e, m=m, i=i: e.dma_start(out=XT[m * 128:(m + 1) * 128, :], in_=xo[i][:]),
                           b_xo[i], reads=[b_xo[i]], writes=[b_XT[m]])
                tk.barrier()

        def phase_ffn(l, src, last):
            moe = (l % 2 == 1)
            li = l // 2
            nexp = moe_experts if moe else 1
            with ExitStack() as st:
                xt = sb(st, "xt", 16 * 512)
                b_xt = [B("xt%d" % k) for k in range(16)]
                h2 = sb(st, "h2", 16 * 512, BF16)
                b_h2 = [B("h2_%d" % k) for k in range(16)]
                act = sb(st, "act", NJ * 512, BF16)
                b_act = [B("act%d" % j) for j in range(NJ)]
                wgb = [sb(st, "wgb%d" % i, 2048, BF16) for i in range(2)]
                b_wgb = [B("wgb0"), B("wgb1")]
                wub = [sb(st, "wub%d" % i, 2048, BF16) for i in range(2)]
                b_wub = [B("wub0"), B("wub1")]
                wdb = [sb(st, "wdb%d" % i, NJ * 128, BF16) for i in range(2)]
                b_wdb = [B("wdb0"), B("wdb1")]
                sqt = [sb(st, "fsq%d" % i, 512) for i in range(2)]
                b_sqt = [B("fsq0"), B("fsq1")]
                rsB = sb(st, "frsB", 512)
                b_rsB = B("frsB")
                tmp = [sb(st, "ftmp%d" % i, 512) for i in range(2)]
                b_tmp = [B("ftmp0"), B("ftmp1")]
                sgt = [sb(st, "fsg%d" % i, 512) for i in range(2)]
                b_sgt = [B("fsg0"), B("fsg1")]
                if moe:
                    h2f = [sb(st, "h2f%d" % i, 512) for i in range(2)]
                    b_h2f = [B("h2f0"), B("h2f1")]
                    lg = sb(st, "lg", 32)
                    b_lg = B("lg")
                    m8 = sb(st, "fm8", 32)
                    b_m8 = B("fm8")
                    rsel = sb(st, "rsel", 32)
                    b_rsel = B("rsel")
                    ex = sb(st, "ex", 32)
                    b_ex = B("ex")
                    rsm = sb(st, "rsm", 16)
                    b_rsm = B("rsm")
                    GT = sb(st, "GT", 32, BF16)
                    b_GT = B("GT")
                    GR = sb(st, "GR", 512, BF16, parts=8)
                    b_GR = B("GR")
                    gB = [sb(st, "gB%d" % i, 512) for i in range(2)]
                    b_gB = [B("gB0"), B("gB1")]
                wcnt = [0, 0]

                def stats(pn):
                    for k in range(16):
                        i = k % 2
                        A(lambda e, k=k, i=i: e.activation(out=sqt[i][:], in_=xt[:, k * 512:(k + 1) * 512],
                                                           func=AF.Square), [b_xt[k]], [b_sqt[i]])
                        mm(ps[pn][:], onesf[:], sqt[i][:], k == 0, k == 15, [b_onesf, b_sqt[i]], [bps[pn]], sig=True)
                    A(lambda e: e.activation(out=rsB[:], in_=ps[pn][:], func=AF.Sqrt, bias=epsc[:], scale=1.0 / D),
                      [bps[pn], b_epsc], [b_rsB])
                    V(lambda e: e.reciprocal(out=rsB[:], in_=rsB[:]), [b_rsB], [b_rsB])

                for tI in range(4):
                    tsl = slice(tI * 512, (tI + 1) * 512)
                    for k in range(16):
                        tk.dma("sp", lambda e, k=k: e.dma_start(out=xt[:, k * 512:(k + 1) * 512],
                                                                in_=src[k * 128:(k + 1) * 128, tsl]),
                               b_xt[k], reads=[b_XT[k]], writes=[b_xt[k]])
                    stats(4)
                    pR = 5
                    if moe:
                        V(lambda e: e.memset(ps[pR][:, 0:32], 0.0), [], [bps[pR]])
                    for k in range(16):
                        i = k % 2
                        V(lambda e, k=k, i=i: e.tensor_tensor(out=tmp[i][:], in0=xt[:, k * 512:(k + 1) * 512],
                                                              in1=rsB[:], op=ALU.mult), [b_xt[k], b_rsB], [b_tmp[i]])
                        A(lambda e, k=k, i=i: e.activation(out=h2[:, k * 512:(k + 1) * 512], in_=tmp[i][:],
                                                           func=AF.Identity, bias=mcol(l, 3, k),
                                                           scale=A2[:, l * 16 + k:l * 16 + k + 1]),
                          [b_tmp[i], b_modT, b_A], [b_h2[k]])
                        if moe:
                            A(lambda e, k=k, i=i: e.activation(out=h2f[i][:], in_=tmp[i][:], func=AF.Identity,
                                                               bias=mcol(l, 3, k),
                                                               scale=A2[:, l * 16 + k:l * 16 + k + 1]),
                              [b_tmp[i], b_modT, b_A], [b_h2f[i]])
                            for tq in range(4):
                                c0 = (li * 16 + k) * 8
                                P(lambda e, tq=tq, i=i, c0=c0, k=k: e.matmul(
                                    ps[pR][:, tq * 8:(tq + 1) * 8], h2f[i][:, tq * 128:(tq + 1) * 128],
                                    rw_s[:, c0:c0 + 8], start=False, stop=(k == 15), skip_group_check=True),
                                  [b_h2f[i], b_rw], [bps[pR]], tq == 3)
                    if moe:
                        V(lambda e: e.tensor_tensor(out=lg[:], in0=ps[pR][:, 0:32], in1=rb_s[:, li * 32:(li + 1) * 32],
                                                    op=ALU.add), [bps[pR], b_rb], [b_lg])
                        for tq in range(4):
                            s8 = slice(tq * 8, (tq + 1) * 8)
                            V(lambda e, s8=s8: e.max(out=m8[:, s8], in_=lg[:, s8]), [b_lg], [b_m8])
                            V(lambda e, s8=s8, tq=tq: e.tensor_scalar(out=rsel[:, s8], in0=lg[:, s8],
                                                                      scalar1=m8[:, tq * 8 + 1:tq * 8 + 2],
                                                                      scalar2=None, op0=ALU.is_ge),
                              [b_lg, b_m8], [b_rsel])
                            V(lambda e, tq=tq: e.tensor_scalar(out=rsm[:, tq:tq + 1], in0=m8[:, tq * 8:tq * 8 + 1],
                                                               scalar1=-1.0, scalar2=None, op0=ALU.mult),
                              [b_m8], [b_rsm])
                            A(lambda e, s8=s8, tq=tq: e.activation(out=ex[:, s8], in_=lg[:, s8], func=AF.Exp,
                                                                   bias=rsm[:, tq:tq + 1], scale=1.0),
                              [b_lg, b_rsm], [b_ex])
                            V(lambda e, s8=s8: e.tensor_tensor(out=ex[:, s8], in0=ex[:, s8], in1=rsel[:, s8],
                                                               op=ALU.mult), [b_ex, b_rsel], [b_ex])
                            V(lambda e, s8=s8, tq=tq: e.tensor_reduce(out=rsm[:, 4 + tq:5 + tq], in_=ex[:, s8],
                                                                      axis=AX.X, op=ALU.add), [b_ex], [b_rsm])
                            V(lambda e, tq=tq: e.reciprocal(out=rsm[:, 8 + tq:9 + tq], in_=rsm[:, 4 + tq:5 + tq]),
                              [b_rsm], [b_rsm])
                            V(lambda e, s8=s8, tq=tq: e.tensor_scalar(out=GT[:, s8], in0=ex[:, s8],
                                                                      scalar1=rsm[:, 8 + tq:9 + tq], scalar2=None,
                                                                      op0=ALU.mult), [b_ex, b_rsm], [b_GT])
                        for tq in range(4):
                            P(lambda e, tq=tq: e.transpose(psb[0:8, tq * 128:(tq + 1) * 128],
                                                           GT[:, tq * 8:(tq + 1) * 8], identb[:]),
                              [b_GT, b_identb], [bpsb[0]], tq == 3)
                        A(lambda e: e.activation(out=GR[:], in_=psb[0:8, 0:512], func=AF.Copy), [bpsb[0]], [b_GR])

                    for ex_i in range(nexp):
                        if moe:
                            gi = ex_i % 2
                            pgb = nextps()
                            mm(ps[pgb][:], e8[:, ex_i * 128:(ex_i + 1) * 128], GR[:], True, True, [b_e8, b_GR],
                               [bps[pgb]])
                            A(lambda e, gi=gi, pgb=pgb: e.activation(out=gB[gi][:], in_=ps[pgb][:], func=AF.Copy),
                              [bps[pgb]], [b_gB[gi]])
                        for j in range(NJ):
                            i = wcnt[0] % 2
                            wcnt[0] += 1
                            gsrc = mog[li, ex_i, j] if moe else ffg[li, j]
                            usrc = mou[li, ex_i, j] if moe else ffu[li, j]
                            tk.dma("pool", lambda e, i=i, gsrc=gsrc: e.dma_start(out=wgb[i][:], in_=gsrc), b_wgb[i],
                                   writes=[b_wgb[i]])
                            tk.dma("pool", lambda e, i=i, usrc=usrc: e.dma_start(out=wub[i][:], in_=usrc), b_wub[i],
                                   writes=[b_wub[i]])
                            pg = nextps()
                            for k in range(16):
                                mm(ps[pg][:], wgb[i][:, k * 128:(k + 1) * 128], h2[:, k * 512:(k + 1) * 512], k == 0,
                                   k == 15, [b_wgb[i], b_h2[k]], [bps[pg]])
                            pu = nextps()
                            for k in range(16):
                                mm(ps[pu][:], wub[i][:, k * 128:(k + 1) * 128], h2[:, k * 512:(k + 1) * 512], k == 0,
                                   k == 15, [b_wub[i], b_h2[k]], [bps[pu]])
                            si = j % 2
                            A(lambda e, si=si, pg=pg: e.activation(out=sgt[si][:], in_=ps[pg][:], func=AF.Silu),
                              [bps[pg]], [b_sgt[si]])
                            V(lambda e, si=si, pu=pu, j=j: e.tensor_tensor(out=act[:, j * 512:(j + 1) * 512],
                                                                           in0=sgt[si][:], in1=ps[pu][:],
                                                                           op=ALU.mult),
                              [b_sgt[si], bps[pu]], [b_act[j]])
                        for m in range(16):
                            i = wcnt[1] % 2
                            wcnt[1] += 1
                            dsrc = mod_[li, ex_i, m] if moe else ffd[li, m]
                            tk.dma("pool", lambda e, i=i, dsrc=dsrc: e.dma_start(out=wdb[i][:], in_=dsrc), b_wdb[i],
                                   writes=[b_wdb[i]])
                            pd = nextps()
                            for j in range(NJ):
                                mm(ps[pd][:], wdb[i][:, j * 128:(j + 1) * 128], act[:, j * 512:(j + 1) * 512], j == 0,
                                   j == NJ - 1, [b_wdb[i], b_act[j]], [bps[pd]])
                            if not moe:
                                V(lambda e, pd=pd, m=m: e.scalar_tensor_tensor(
                                    out=xt[:, m * 512:(m + 1) * 512], in0=ps[pd][:], scalar=mcol(l, 5, m),
                                    in1=xt[:, m * 512:(m + 1) * 512], op0=ALU.mult, op1=ALU.add),
                                  [bps[pd], b_modT, b_xt[m]], [b_xt[m]])
                            else:
                                ti = m % 2
                                V(lambda e, pd=pd, m=m, ti=ti, gi=gi: e.scalar_tensor_tensor(
                                    out=tmp[ti][:], in0=ps[pd][:], scalar=mcol(l, 5, m), in1=gB[gi][:],
                                    op0=ALU.mult, op1=ALU.mult), [bps[pd], b_modT, b_gB[gi]], [b_tmp[ti]])
                                V(lambda e, m=m, ti=ti: e.tensor_tensor(
                                    out=xt[:, m * 512:(m + 1) * 512], in0=xt[:, m * 512:(m + 1) * 512],
                                    in1=tmp[ti][:], op=ALU.add), [b_xt[m], b_tmp[ti]], [b_xt[m]])
                    if not last:
                        for k in range(16):
                            tk.dma("sp", lambda e, k=k: e.dma_start(out=XT[k * 128:(k + 1) * 128, tsl],
                                                                    in_=xt[:, k * 512:(k + 1) * 512]),
                                   b_xt[k], reads=[b_xt[k]], writes=[b_XT[k]])
                    else:
                        stats(4)
                        for k in range(16):
                            i = k % 2
                            V(lambda e, k=k, i=i: e.scalar_tensor_tensor(
                                out=tmp[i][:], in0=xt[:, k * 512:(k + 1) * 512], scalar=gfin_s[:, k:k + 1], in1=rsB[:],
                                op0=ALU.mult, op1=ALU.mult), [b_xt[k], b_gfin, b_rsB], [b_tmp[i]])
                            tk.dma("sp", lambda e, k=k, i=i: e.dma_start(out=outT[k * 128:(k + 1) * 128, tsl],
                                                                         in_=tmp[i][:]),
                                   b_tmp[i], reads=[b_tmp[i]], writes=[b_XT[k]])
                tk.barrier()

        src = xT_in
        for l in range(nl):
            if do_mix:
                with ExitStack() as sth:
                    HTh[0] = sb(sth, "HT", 16 * T, BF16)
                    phase_norm1(l, src)
                    phase_mixer(l)
                    phase_outproj(l, src)
                    tk.barrier()
                src = XT
            if do_ffn:
                phase_ffn(l, src, last=(l == nl - 1))
                src = XT
        tk.barrier()
        build_program.stats = dict(tk.ninst)
    return nc


def _ptab(v, nchunk):
    return np.ascontiguousarray(np.asarray(v, np.float32).reshape(nchunk, 128).T)


def _wtile(w, cols):
    K, n = w.shape
    nb = n // cols
    kc = K // 128
    return np.ascontiguousarray(w.reshape(kc, 128, nb, cols).transpose(2, 1, 0, 3).reshape(nb, 128, kc * cols))


def _constants():
    bf = ml_dtypes.bfloat16
    c = {}
    c["c_identb"] = np.eye(128, dtype=np.float32).astype(bf)
    c["c_ones"] = np.ones((128, 128), np.float32)
    tri = np.triu(np.ones((128, 128), np.float32))
    c["c_tri"] = tri
    c["c_trib"] = tri.astype(bf)
    es = np.zeros((10, 8, 128), np.float32)
    es[0, :, :] = 1.0
    es[1, :, :] = 1.0
    for n in range(8):
        es[2 + n, n, :] = 1.0
    c["c_esel"] = es.reshape(10, 8 * 128).astype(bf)
    e8 = np.zeros((8, 8, 128), np.float32)
    for n in range(8):
        e8[n, n, :] = 1.0
    c["c_e8"] = e8.reshape(8, 8 * 128).astype(bf)
    slopes = 2.0 ** (-(np.arange(1, 9, dtype=np.float64)))
    pos = np.arange(2048, dtype=np.float64).reshape(16, 128)
    al = np.zeros((128, 8, 16, 2), np.float32)
    for h in range(8):
        v = (-slopes[h] * pos).T
        hi = v.astype(np.float32).astype(bf)
        lo = (v - hi.astype(np.float64)).astype(np.float32).astype(bf)
        al[:, h, :, 0] = hi.astype(np.float32)
        al[:, h, :, 1] = lo.astype(np.float32)
    c["c_alibi"] = al.reshape(128, 8 * 16 * 2).astype(bf)
    kp = np.zeros((128, 8, 16), np.float32)
    for h in range(8):
        kp[:, h, :] = (slopes[h] * pos).T
    c["c_kpb"] = kp.reshape(128, 128)
    pb = np.zeros((128, 16, 8), np.float32)
    p01 = np.zeros((128, 16, 8), np.float32)
    o01 = np.zeros((128, 16, 8), np.float32)
    for tt in range(16):
        qb = tt // 2
        for n in range(8):
            if n < qb:
                p01[:, tt, n] = 1.0
            else:
                pb[:, tt, n] = -1e30
            if n == qb:
                o01[:, tt, n] = 1.0
    c["c_pastbias"] = pb.reshape(128, 128)
    c["c_past01"] = p01.reshape(128, 128)
    c["c_own01"] = o01.reshape(128, 128)
    return c


def prep_shared(inp):
    f = lambda a: np.asarray(a, np.float32)
    sh = {}
    w_mod = f(inp["w_mod"])
    sh["wmod"] = np.ascontiguousarray(
        w_mod.reshape(NL, 16, 128, 24, 512).transpose(0, 3, 2, 1, 4).reshape(NL, 24, 128, 16 * 512))
    sh["bmodT"] = np.concatenate([_ptab(f(inp["b_mod"])[l], 96) for l in range(NL)], axis=1)
    sh["gmixT"] = np.concatenate([_ptab(f(inp["g_mix"])[l], 16) for l in range(NL)], axis=1)
    sh["gffnT"] = np.concatenate([_ptab(f(inp["g_ffn"])[l], 16) for l in range(NL)], axis=1)
    sh["gfinT"] = _ptab(f(inp["g_final"]), 16)
    w_in = f(inp["w_in"])
    fm_cols = np.concatenate([np.arange(0, 2048), np.arange(3080, 5128)])
    tm_cols = np.concatenate([np.arange(2048, 3072), np.arange(5128, 6152)])
    sh["winF"] = np.stack([_wtile(np.ascontiguousarray(w_in[l][:, fm_cols]), 128) for l in range(NL)])
    sh["winT"] = np.stack([_wtile(np.ascontiguousarray(w_in[l][:, tm_cols]), 128) for l in range(NL)])
    sh["winG"] = np.stack([_wtile(np.ascontiguousarray(w_in[l][:, 3072:3080]), 8)[0] for l in range(NL)])
    cw = f(inp["conv_w"])
    sh["convw"] = np.ascontiguousarray(
        cw.reshape(NL, 31, 4, 128).transpose(3, 0, 2, 1).reshape(128, NL * 4 * 31))
    cp = np.stack([f(inp["conv_b"]), f(inp["conv_ln_g"]), f(inp["conv_ln_b"])], axis=-1)
    sh["convp"] = np.ascontiguousarray(cp.reshape(NL, 4, 128, 3).transpose(2, 0, 1, 3).reshape(128, NL * 4 * 3))
    mw = f(inp["ml_conv_w"])
    sh["mlcw"] = np.ascontiguousarray(mw.reshape(NL, 4, 8, 128).transpose(3, 0, 2, 1).reshape(128, NL * 8 * 4))
    sh["mlcb"] = np.ascontiguousarray(f(inp["ml_conv_b"]).reshape(NL, 8, 128).transpose(2, 0, 1).reshape(128, NL * 8))
    bif = np.concatenate([f(inp["ml_b_i"]), f(inp["ml_b_f"])], axis=1)
    sh["mlbif"] = np.ascontiguousarray(np.broadcast_to(bif[None, :, None, :], (128, NL, 16, 8)).reshape(128, NL * 128))
    sh["mlng"] = np.ascontiguousarray(f(inp["ml_norm_g"]).reshape(NL, 4, 128).transpose(2, 0, 1).reshape(128, NL * 4))
    sh["atng"] = np.ascontiguousarray(f(inp["attn_norm_g"]).reshape(NL, 8, 128).transpose(2, 0, 1).reshape(128, NL * 8))
    sh["wout"] = np.stack([_wtile(f(inp["w_out"])[l], 128) for l in range(NL)])
    sh["ffg"] = np.stack([_wtile(f(inp["ffn_w_gate"])[i], 128) for i in range(2)])
    sh["ffu"] = np.stack([_wtile(f(inp["ffn_w_up"])[i], 128) for i in range(2)])

    def dtile(w):
        return np.ascontiguousarray(w.reshape(NJ, 128, 16, 128).transpose(2, 1, 0, 3).reshape(16, 128, NJ * 128))

    sh["ffd"] = np.stack([dtile(f(inp["ffn_w_down"])[i]) for i in range(2)])
    sh["mog"] = np.stack([np.stack([_wtile(f(inp["moe_w_gate"])[i, e], 128) for e in range(NE)]) for i in range(2)])
    sh["mou"] = np.stack([np.stack([_wtile(f(inp["moe_w_up"])[i, e], 128) for e in range(NE)]) for i in range(2)])
    sh["mod"] = np.stack([np.stack([dtile(f(inp["moe_w_down"])[i, e]) for e in range(NE)]) for i in range(2)])
    rwt = f(inp["moe_w_router"])
    sh["rw"] = np.ascontiguousarray(rwt.reshape(2, 16, 128, 8).transpose(2, 0, 1, 3).reshape(128, 2 * 16 * 8))
    rbt = f(inp["moe_b_router"])
    sh["rb"] = np.ascontiguousarray(np.broadcast_to(rbt[None, :, None, :], (128, 2, 4, 8)).reshape(128, 64))
    sh.update(_constants())
    return sh


def kernel(**inputs):
    x = np.asarray(inputs["x"], np.float32)
    c = np.asarray(inputs["c"], np.float32)
    B = x.shape[0]
    sh = prep_shared(inputs)
    nc = build_program()
    in_maps = []
    for b in range(B):
        m = dict(sh)
        m["xT"] = np.ascontiguousarray(x[b].T)
        m["condT"] = _ptab(c[b], 16)
        in_maps.append(m)
    res = run_bass_kernel_spmd(nc, in_maps, core_ids=list(range(B)))
    out = np.stack([np.ascontiguousarray(np.asarray(r["outT"], np.float32).T) for r in res.results])
    return out
```

```python
import numpy as np
import ml_dtypes
import concourse.bass as bass
import concourse.mybir as mybir
from concourse.bass_utils import run_bass_kernel_spmd
from contextlib import ExitStack

F32 = mybir.dt.float32
BF16 = mybir.dt.bfloat16
AF = mybir.ActivationFunctionType
ALU = mybir.AluOpType
AX = mybir.AxisListType

T = 2048
D = 2048
NL = 4
DFF = 5632
NJ = 44
NE = 8
EPS = 1e-6
BIG = 30000.0
ENGS = ("pe", "act", "dve", "pool", "sp")


class Buf:
    __slots__ = ("name", "lw", "rd", "sem", "dcount", "excl")

    def __init__(self, name, excl=False):
        self.name = name
        self.excl = excl
        self.lw = None
        self.rd = []
        self.sem = None
        self.dcount = 0


class TK:
    def __init__(self, nc, es):
        self.nc = nc
        self.es = es
        self.eobj = {"pe": nc.tensor, "act": nc.scalar, "dve": nc.vector, "pool": nc.gpsimd, "sp": nc.sync}
        self.esem = {}
        self.ecount = {e: 0 for e in ENGS}
        self.known = {e: {} for e in ENGS}
        self.sems = {}
        self.dsems = {}
        self.nsem = 0
        self.ninst = {e: 0 for e in ENGS}
        for e in ENGS:
            self.esem[e] = self.new_sem("e_" + e)

    def new_sem(self, name):
        s = self.es.enter_context(self.nc.semaphore(name))
        self.nsem += 1
        key = "s%d" % self.nsem
        self.sems[key] = s
        return key

    def _need(self, eng, t, waits):
        if t is None:
            return
        key, val, src = t
        if self.known[eng].get(key, 0) >= val:
            return
        if waits.get(key, 0) < val:
            waits[key] = val

    def _emit_waits(self, eng, waits):
        for key, val in waits.items():
            self.eobj[eng].wait_ge(self.sems[key], val)
            self.known[eng][key] = val
            self.ninst[eng] += 1

    def op(self, eng, fn, reads=(), writes=(), signal=True):
        writes = list(writes) + [b for b in reads if b.excl]
        reads = [b for b in reads if not b.excl]
        waits = {}
        for b in reads:
            self._need(eng, b.lw, waits)
        for b in writes:
            if b.lw is not None and b.lw[2] != eng:
                self._need(eng, b.lw, waits)
            for t in b.rd:
                if t[2] != eng:
                    self._need(eng, t, waits)
        self._emit_waits(eng, waits)
        key = self.esem[eng]
        val = self.ecount[eng] + 1
        t = (key, val, eng)
        for b in reads:
            b.rd.append(t)
        for b in writes:
            b.lw = t
            b.rd = []
        ins = fn(self.eobj[eng])
        self.ninst[eng] += 1
        if signal:
            self.ecount[eng] = val
            ins.then_inc(self.sems[key], 1)

    def dma(self, eng, fn, sb, reads=(), writes=()):
        if sb.sem is None:
            sb.sem = self.new_sem("d_" + sb.name)
            self.dsems[sb.sem] = sb
        waits = {}
        for b in reads:
            self._need(eng, b.lw, waits)
        for b in writes:
            self._need(eng, b.lw, waits)
            for t in b.rd:
                self._need(eng, t, waits)
        self._emit_waits(eng, waits)
        sb.dcount += 16
        t = (sb.sem, sb.dcount, None)
        for b in reads:
            b.rd.append(t)
        for b in writes:
            b.lw = t
            b.rd = []
        fn(self.eobj[eng]).then_inc(self.sems[sb.sem], 16)
        self.ninst[eng] += 1

    def barrier(self, engs=ENGS):
        for e in engs:
            waits = {}
            for p in ENGS:
                if self.ecount[p] > 0:
                    self._need(e, (self.esem[p], self.ecount[p], p), waits)
            for key, sb in self.dsems.items():
                if sb.dcount > 0:
                    self._need(e, (key, sb.dcount, None), waits)
            self._emit_waits(e, waits)


def build_program(nl=NL, do_mix=True, do_ffn=True, dbg=False, moe_experts=NE, mixers=("conv", "ml", "att"),
                  nlw=NL, ndw=2, nmw=2):
    nc = bass.Bass("TRN2", target_bir_lowering=False)

    def din(name, shape, dt=F32):
        return nc.dram_tensor(name, list(shape), dt, kind="ExternalInput").ap()

    xT_in = din("xT", [D, T])
    condT = din("condT", [128, 16])
    wmod = din("wmod", [nlw, 24, 128, 16 * 512])
    bmodT = din("bmodT", [128, NL * 96])
    gmixT = din("gmixT", [128, NL * 16])
    gffnT = din("gffnT", [128, NL * 16])
    gfinT = din("gfinT", [128, 16])
    winF = din("winF", [nlw, 32, 128, 2048])
    winT = din("winT", [nlw, 16, 128, 2048])
    winG = din("winG", [NL, 128, 128])
    convw = din("convw", [128, NL * 4 * 31])
    convp = din("convp", [128, NL * 4 * 3])
    mlcw = din("mlcw", [128, NL * 8 * 4])
    mlcb = din("mlcb", [128, NL * 8])
    mlbif = din("mlbif", [128, NL * 128])
    mlng = din("mlng", [128, NL * 4])
    atng = din("atng", [128, NL * 8])
    wout = din("wout", [nlw, 16, 128, 2048])
    ffg = din("ffg", [ndw, NJ, 128, 2048])
    ffu = din("ffu", [ndw, NJ, 128, 2048])
    ffd = din("ffd", [ndw, 16, 128, NJ * 128])
    mog = din("mog", [nmw, NE, NJ, 128, 2048] if nmw else [1, 1, 1, 128, 2048])
    mou = din("mou", [nmw, NE, NJ, 128, 2048] if nmw else [1, 1, 1, 128, 2048])
    mod_ = din("mod", [nmw, NE, 16, 128, NJ * 128] if nmw else [1, 1, 1, 128, NJ * 128])
    rw = din("rw", [128, 2 * 16 * 8])
    rb = din("rb", [128, 2 * 32])
    c_identb = din("c_identb", [128, 128], BF16)
    c_ones = din("c_ones", [128, 128])
    c_tri = din("c_tri", [128, 128])
    c_trib = din("c_trib", [128, 128], BF16)
    c_esel = din("c_esel", [10, 8 * 128], BF16)
    c_e8 = din("c_e8", [8, 8 * 128], BF16)
    c_alibi = din("c_alibi", [128, 8 * 16 * 2], BF16)
    c_kpb = din("c_kpb", [128, 8 * 16])
    c_pastbias = din("c_pastbias", [128, 128])
    c_past01 = din("c_past01", [128, 128])
    c_own01 = din("c_own01", [128, 128])

    okind = "ExternalOutput"
    outT = nc.dram_tensor("outT", [D, T], F32, kind=okind).ap()
    skind = "ExternalOutput" if dbg else "Internal"
    XT = nc.dram_tensor("XT", [D, T], F32, kind=skind).ap()
    YT = nc.dram_tensor("YT", [D, T], BF16, kind=skind).ap()

    with ExitStack() as es:
        tk = TK(nc, es)
        bufs = {}

        def B(name, excl=False):
            if name not in bufs:
                bufs[name] = Buf(name, excl)
            return bufs[name]

        uid = [0]

        def sb(st, name, cols, dt=F32, parts=128):
            uid[0] += 1
            return st.enter_context(nc.sbuf_tensor("%s_%d" % (name, uid[0]), [parts, cols], dt))

        def V(fn, r, w, sig=True):
            tk.op("dve", fn, r, w, sig)

        def A(fn, r, w, sig=True):
            tk.op("act", fn, r, w, sig)

        def G(fn, r, w, sig=True):
            tk.op("pool", fn, r, w, sig)

        def P(fn, r, w, sig=True):
            tk.op("pe", fn, r, w, sig)

        def mm(out, lhsT, rhs, start, stop, r, w, sig=None, skip=False):
            if skip:
                P(lambda e: e.matmul(out, lhsT, rhs, start=start, stop=stop, skip_group_check=True), r, w,
                  stop if sig is None else sig)
            else:
                P(lambda e: e.matmul(out, lhsT, rhs, start=start, stop=stop), r, w, stop if sig is None else sig)

        NPS = 6
        ps = [es.enter_context(nc.psum_tensor("ps%d" % i, [128, 512], F32)) for i in range(NPS)]
        bps = [B("ps%d" % i, True) for i in range(NPS)]
        psb = es.enter_context(nc.psum_tensor("psb", [128, 2048], BF16))
        bpsb = [B("psb%d" % (i // 2), True) for i in range(4)]
        wk = [0]

        def nextps(n=4):
            i = wk[0] % n
            wk[0] += 1
            return i

        def load_const(name, src, cols, dt=F32, parts=128):
            t = sb(es, name, cols, dt, parts)
            b = B(name)
            tk.dma("sp", lambda e: e.dma_start(out=t[:], in_=src), b, writes=[b])
            return t, b

        identb, b_identb = load_const("identb", c_identb, 128, BF16)
        onesf, b_onesf = load_const("onesf", c_ones, 128)
        trif, b_trif = load_const("trif", c_tri, 128)
        trib, b_trib = load_const("tribb", c_trib, 128, BF16)
        esel, b_esel = load_const("esel", c_esel, 8 * 128, BF16, parts=10)
        e8, b_e8 = load_const("e8", c_e8, 8 * 128, BF16, parts=8)
        alibi, b_alibi = load_const("alibi", c_alibi, 8 * 16 * 2, BF16)
        kpb, b_kpb = load_const("kpb", c_kpb, 128)
        pastbias, b_pastbias = load_const("pastbias", c_pastbias, 128)
        past01, b_past01 = load_const("past01", c_past01, 128)
        own01, b_own01 = load_const("own01", c_own01, 128)
        bmod_s, b_bmod = load_const("bmods", bmodT, NL * 96)
        gmix_s, b_gmix = load_const("gmixs", gmixT, NL * 16)
        gffn_s, b_gffn = load_const("gffns", gffnT, NL * 16)
        gfin_s, b_gfin = load_const("gfins", gfinT, 16)
        convw_s, b_convw = load_const("convws", convw, NL * 4 * 31)
        convp_s, b_convp = load_const("convps", convp, NL * 4 * 3)
        mlcw_s, b_mlcw = load_const("mlcws", mlcw, NL * 8 * 4)
        mlcb_s, b_mlcb = load_const("mlcbs", mlcb, NL * 8)
        mlbif_s, b_mlbif = load_const("mlbifs", mlbif, NL * 128)
        mlng_s, b_mlng = load_const("mlngs", mlng, NL * 4)
        atng_s, b_atng = load_const("atngs", atng, NL * 8)
        rw_s, b_rw = load_const("rws", rw, 2 * 16 * 8)
        rb_s, b_rb = load_const("rbs", rb, 2 * 32)
        meanf = sb(es, "meanf", 128)
        b_meanf = B("meanf")
        V(lambda e: e.tensor_scalar(out=meanf[:], in0=onesf[:], scalar1=1.0 / 128.0, scalar2=None, op0=ALU.mult),
          [b_onesf], [b_meanf])
        onesb = sb(es, "onesb", 128, BF16)
        b_onesb = B("onesb")
        V(lambda e: e.tensor_copy(out=onesb[:], in_=onesf[:]), [b_onesf], [b_onesb])
        epsc = sb(es, "epsc", 1)
        b_epsc = B("epsc")
        V(lambda e: e.memset(epsc[:], EPS), [], [b_epsc])
        onec = sb(es, "onec", 1)
        b_onec = B("onec")
        V(lambda e: e.memset(onec[:], 1.0), [], [b_onec])

        modT = sb(es, "modT", NL * 96)
        b_modT = B("modT")
        A1 = sb(es, "A1", NL * 16)
        A2 = sb(es, "A2", NL * 16)
        b_A = B("A12")

        with ExitStack() as st:
            cT = sb(st, "cT", 16)
            b_cT = B("cT")
            tk.dma("sp", lambda e: e.dma_start(out=cT[:], in_=condT), b_cT, writes=[b_cT])
            cS = sb(st, "cS", 16)
            b_cS = B("cS")
            A(lambda e: e.activation(out=cS[:], in_=cT[:], func=AF.Silu), [b_cT], [b_cS])
            wm = [sb(st, "wm%d" % i, 16 * 512) for i in range(2)]
            b_wm = [B("wm0"), B("wm1")]
            rowb = [sb(st, "rowb%d" % i, 512, F32, parts=1) for i in range(2)]
            b_rowb = [B("rowb0"), B("rowb1")]
            for l in range(nl):
                pi = 5
                for cb in range(24):
                    w_ = cb % 2
                    tk.dma("sp", lambda e, l=l, cb=cb, w_=w_: e.dma_start(out=wm[w_][:], in_=wmod[l, cb]),
                           b_wm[w_], writes=[b_wm[w_]])
                    pr = nextps()
                    for k in range(16):
                        mm(ps[pr][0:1, :], cS[:, k:k + 1], wm[w_][:, k * 512:(k + 1) * 512], k == 0, k == 15,
                           [b_wm[w_], b_cS], [bps[pr]])
                    A(lambda e, w_=w_, pr=pr: e.activation(out=rowb[w_][:], in_=ps[pr][0:1, :], func=AF.Copy),
                      [bps[pr]], [b_rowb[w_]])
                    for c4 in range(4):
                        j = cb * 4 + c4
                        mm(ps[pi][:, j:j + 1], rowb[w_][0:1, c4 * 128:(c4 + 1) * 128], onec[0:1, 0:1], True, True,
                           [b_rowb[w_], b_onec], [bps[pi]], sig=(c4 == 3))
                V(lambda e, l=l, pi=pi: e.tensor_tensor(out=modT[:, l * 96:(l + 1) * 96], in0=ps[pi][:, 0:96],
                                                        in1=bmod_s[:, l * 96:(l + 1) * 96], op=ALU.add),
                  [bps[pi], b_bmod], [b_modT])
                V(lambda e, l=l: e.scalar_tensor_tensor(out=A1[:, l * 16:(l + 1) * 16],
                                                        in0=modT[:, l * 96 + 16:l * 96 + 32], scalar=1.0,
                                                        in1=gmix_s[:, l * 16:(l + 1) * 16], op0=ALU.add, op1=ALU.mult),
                  [b_modT, b_gmix], [b_A])
                V(lambda e, l=l: e.scalar_tensor_tensor(out=A2[:, l * 16:(l + 1) * 16],
                                                        in0=modT[:, l * 96 + 64:l * 96 + 80], scalar=1.0,
                                                        in1=gffn_s[:, l * 16:(l + 1) * 16], op0=ALU.add, op1=ALU.mult),
                  [b_modT, b_gffn], [b_A])
            tk.barrier()

        def mcol(l, which, k):
            c = l * 96 + which * 16 + k
            return modT[:, c:c + 1]

        b_XT = [B("XT%d" % k) for k in range(16)]
        b_YT = [B("YT%d" % k) for k in range(16)]

        HTh = [None]
        b_HT = [B("HT%d" % k) for k in range(16)]

        def ht(k, a, b):
            return HTh[0][:, k * T + a:k * T + b]

        def phase_norm1(l, src):
            with ExitStack() as st:
                xk = [sb(st, "xk%d" % i, T) for i in range(2)]
                b_xk = [B("xk0"), B("xk1")]
                sq = [sb(st, "sq%d" % i, T, BF16) for i in range(2)]
                b_sq = [B("sq0"), B("sq1")]
                rsB = sb(st, "rsB", T)
                b_rsB = B("rsB")
                tmp = [sb(st, "tmp%d" % i, T) for i in range(2)]
                b_tmp = [B("tmp0"), B("tmp1")]
                for k in range(16):
                    i = k % 2
                    tk.dma("sp", lambda e, k=k, i=i: e.dma_start(out=xk[i][:], in_=src[k * 128:(k + 1) * 128, :]),
                           b_xk[i], reads=[b_XT[k]], writes=[b_xk[i]])
                    A(lambda e, i=i: e.activation(out=sq[i][:], in_=xk[i][:], func=AF.Square), [b_xk[i]], [b_sq[i]])
                    for t in range(4):
                        mm(ps[t][:], onesb[:], sq[i][:, t * 512:(t + 1) * 512], k == 0, k == 15,
                           [b_onesb, b_sq[i]], [bps[t]], sig=(k == 15 or t == 3))
                for t in range(4):
                    A(lambda e, t=t: e.activation(out=rsB[:, t * 512:(t + 1) * 512], in_=ps[t][:], func=AF.Sqrt,
                                                  bias=epsc[:], scale=1.0 / D), [bps[t], b_epsc], [b_rsB])
                V(lambda e: e.reciprocal(out=rsB[:], in_=rsB[:]), [b_rsB], [b_rsB])
                for k in range(16):
                    i = k % 2
                    tk.dma("sp", lambda e, k=k, i=i: e.dma_start(out=xk[i][:], in_=src[k * 128:(k + 1) * 128, :]),
                           b_xk[i], reads=[b_XT[k]], writes=[b_xk[i]])
                    V(lambda e, i=i: e.tensor_tensor(out=tmp[i][:], in0=xk[i][:], in1=rsB[:], op=ALU.mult),
                      [b_xk[i], b_rsB], [b_tmp[i]])
                    A(lambda e, k=k, i=i: e.activation(out=ht(k, 0, T), in_=tmp[i][:], func=AF.Identity,
                                                       bias=mcol(l, 0, k), scale=A1[:, l * 16 + k:l * 16 + k + 1]),
                      [b_tmp[i], b_modT, b_A], [b_HT[k]])
                tk.barrier()

        def proj_fm(wb, b_wb, t, pi):
            for k in range(16):
                mm(ps[pi][:], wb[:, k * 128:(k + 1) * 128], ht(k, t * 512, (t + 1) * 512), k == 0, k == 15,
                   [b_wb, b_HT[k]], [bps[pi]])

        def proj_tm(wb, b_wb, tt, out_ap, b_out, ncols=128):
            for k in range(16):
                mm(out_ap, ht(k, tt * 128, (tt + 1) * 128), wb[:, k * ncols:(k + 1) * ncols], k == 0, k == 15,
                   [b_wb, b_HT[k]], [b_out])

        def phase_mixer(l):
            with ExitStack() as st:
                wbs = [sb(st, "wb%d" % i, 2048, BF16) for i in range(3)]
                b_wbs = [B("wb%d" % i) for i in range(3)]
                wi = [0]

                def loadw(src):
                    i = wi[0] % 3
                    wi[0] += 1
                    tk.dma("pool", lambda e: e.dma_start(out=wbs[i][:], in_=src), b_wbs[i], writes=[b_wbs[i]])
                    return wbs[i], b_wbs[i]

                yT = [sb(st, "yT%d" % i, T, BF16) for i in range(2)]
                b_yT = [B("yT0"), B("yT1")]
                yi = [0]

                def store_y(row, i):
                    tk.dma("sp", lambda e: e.dma_start(out=YT[row * 128:(row + 1) * 128, :], in_=yT[i][:]),
                           b_yT[i], reads=[b_yT[i]], writes=[b_YT[row]])

                if "conv" in mixers:
                    with ExitStack() as s2:
                        upad = sb(s2, "upad", 30 + T, BF16)
                        b_upad = B("upad")
                        dg = [sb(s2, "dg%d" % i, 31 * 128, BF16) for i in range(2)]
                        b_dg = [B("dg0"), B("dg1")]
                        acc = sb(s2, "acc", T)
                        b_acc = B("acc")
                        cen = sb(s2, "cen", T)
                        b_cen = B("cen")
                        sq = sb(s2, "csq", T)
                        b_sq = B("csq")
                        sgt = [sb(s2, "sgt%d" % i, 512) for i in range(2)]
                        b_sgt = [B("sgt0"), B("sgt1")]
                        rst = [sb(s2, "rst%d" % i, 512) for i in range(2)]
                        b_rst = [B("rst0"), B("rst1")]
                        V(lambda e: e.memset(upad[:, 0:30], 0.0), [], [b_upad])
                        for j in range(4):
                            wa, b_wa = loadw(winF[l, j])
                            wg_, b_wg = loadw(winF[l, 4 + j])
                            for t in range(4):
                                pa = nextps()
                                proj_fm(wa, b_wa, t, pa)
                                pg = nextps()
                                proj_fm(wg_, b_wg, t, pg)
                                i = t % 2
                                A(lambda e, i=i, pg=pg: e.activation(out=sgt[i][:], in_=ps[pg][:], func=AF.Sigmoid),
                                  [bps[pg]], [b_sgt[i]])
                                V(lambda e, i=i, pa=pa, t=t: e.tensor_tensor(
                                    out=upad[:, 30 + t * 512:30 + (t + 1) * 512], in0=ps[pa][:], in1=sgt[i][:],
                                    op=ALU.mult), [bps[pa], b_sgt[i]], [b_upad])
                            cw0 = (l * 4 + j) * 31
                            cp0 = (l * 4 + j) * 3
                            di = j % 2
                            for jj in range(31):
                                V(lambda e, jj=jj, cw0=cw0, di=di: e.tensor_scalar(
                                    out=dg[di][:, jj * 128:(jj + 1) * 128], in0=identb[:],
                                    scalar1=convw_s[:, cw0 + jj:cw0 + jj + 1], scalar2=None, op0=ALU.mult),
                                  [b_identb, b_convw], [b_dg[di]], sig=(jj == 30))
                            for t in range(4):
                                pc = nextps()
                                for jj in range(31):
                                    mm(ps[pc][:], dg[di][:, jj * 128:(jj + 1) * 128],
                                       upad[:, jj + t * 512:jj + (t + 1) * 512], jj == 0, jj == 30,
                                       [b_dg[di], b_upad], [bps[pc]])
                                A(lambda e, pc=pc, t=t, cp0=cp0: e.activation(
                                    out=acc[:, t * 512:(t + 1) * 512], in_=ps[pc][:], func=AF.Identity,
                                    bias=convp_s[:, cp0:cp0 + 1], scale=1.0), [bps[pc], b_convp], [b_acc])
                            y_i = yi[0] % 2
                            yi[0] += 1
                            for t in range(4):
                                sl = slice(t * 512, (t + 1) * 512)
                                pm = nextps()
                                mm(ps[pm][:], meanf[:], acc[:, sl], True, True, [b_meanf, b_acc], [bps[pm]])
                                V(lambda e, sl=sl, pm=pm: e.tensor_tensor(out=cen[:, sl], in0=acc[:, sl],
                                                                          in1=ps[pm][:], op=ALU.subtract),
                                  [b_acc, bps[pm]], [b_cen])
                                A(lambda e, sl=sl: e.activation(out=sq[:, sl], in_=cen[:, sl], func=AF.Square),
                                  [b_cen], [b_sq])
                                pv = nextps()
                                mm(ps[pv][:], meanf[:], sq[:, sl], True, True, [b_meanf, b_sq], [bps[pv]])
                                i = t % 2
                                A(lambda e, i=i, pv=pv: e.activation(out=rst[i][:], in_=ps[pv][:], func=AF.Sqrt,
                                                                     bias=epsc[:], scale=1.0),
                                  [bps[pv], b_epsc], [b_rst[i]])
                                V(lambda e, i=i: e.reciprocal(out=rst[i][:], in_=rst[i][:]), [b_rst[i]], [b_rst[i]])
                                V(lambda e, i=i, sl=sl: e.tensor_tensor(out=cen[:, sl], in0=cen[:, sl], in1=rst[i][:],
                                                                        op=ALU.mult), [b_cen, b_rst[i]], [b_cen])
                                A(lambda e, sl=sl, y_i=y_i, cp0=cp0: e.activation(
                                    out=yT[y_i][:, sl], in_=cen[:, sl], func=AF.Silu,
                                    bias=convp_s[:, cp0 + 2:cp0 + 3], scale=convp_s[:, cp0 + 1:cp0 + 2]),
                                  [b_cen, b_convp], [b_yT[y_i]])
                            store_y(j, y_i)
                        tk.barrier()

                if "ml" in mixers:
                    with ExitStack() as s2:
                        gpre = sb(s2, "gpre", 128)
                        b_gpre = B("gpre")
                        GI = sb(s2, "GI", 64)
                        EZ = sb(s2, "EZ", 64)
                        LF = sb(s2, "LF", 64)
                        AA = sb(s2, "AA", 64)
                        EB = sb(s2, "EB", 64)
                        EBL = sb(s2, "EBL", 64)
                        b_g = B("gates")
                        b_g2 = B("gates2")
                        wg_ = sb(s2, "wgates", 128, BF16)
                        b_wg = B("wgates")
                        tk.dma("pool", lambda e: e.dma_start(out=wg_[:], in_=winG[l]), b_wg, writes=[b_wg])
                        pg = 4
                        for tt in range(16):
                            proj_tm(wg_, b_wg, tt, ps[pg][:, tt * 8:(tt + 1) * 8], bps[pg], ncols=8)
                        V(lambda e: e.tensor_tensor(out=gpre[:], in0=ps[pg][:, 0:128],
                                                    in1=mlbif_s[:, l * 128:(l + 1) * 128], op=ALU.add),
                          [bps[pg], b_mlbif], [b_gpre])
                        g3 = gpre[:].rearrange("p (t e) -> p t e", e=8)

                        def v3(tl):
                            return tl[:].rearrange("p (t e) -> p t e", e=4)

                        V(lambda e: e.tensor_copy(out=v3(GI), in_=g3[:, :, 0:4]), [b_gpre], [b_g])
                        A(lambda e: e.activation(out=v3(EZ), in_=g3[:, :, 4:8], func=AF.Exp, scale=-1.0),
                          [b_gpre], [b_g])
                        A(lambda e: e.activation(out=LF[:], in_=EZ[:], func=AF.Ln, bias=onec[:], scale=1.0),
                          [b_g, b_onec], [b_g])
                        pB, pL = 4, 5
                        mm(ps[pB][:, 0:64], trif[:], LF[:], True, True, [b_trif, b_g], [bps[pB]])
                        mm(ps[pL][:, 0:64], onesf[:], LF[:], True, True, [b_onesf, b_g], [bps[pL]])
                        V(lambda e: e.tensor_tensor(out=AA[:], in0=GI[:], in1=ps[pB][:, 0:64], op=ALU.add),
                          [b_g, bps[pB]], [b_g2])
                        A(lambda e: e.activation(out=AA[:], in_=AA[:], func=AF.Exp), [b_g2], [b_g2])
                        A(lambda e: e.activation(out=EB[:], in_=ps[pB][:, 0:64], func=AF.Exp, scale=-1.0),
                          [bps[pB]], [b_g2])
                        A(lambda e: e.activation(out=EBL[:], in_=ps[pL][:, 0:64], func=AF.Exp, scale=-1.0),
                          [bps[pL]], [b_g2])

                        qpad = sb(s2, "qpad", 3 + T)
                        b_qpad = B("qpad")
                        cacc = sb(s2, "cacc", T)
                        b_cacc = B("cacc")
                        qT = sb(s2, "qT", T, BF16)
                        b_qT = B("qT")
                        kT = sb(s2, "kT", T, BF16)
                        b_kT = B("kT")
                        ktm = sb(s2, "ktm", T, BF16)
                        b_ktm = B("ktm")
                        va = sb(s2, "va", 16 * 129, BF16)
                        b_va = B("va")
                        og = sb(s2, "og", T, BF16)
                        b_og = B("og")
                        PT = [sb(s2, "mPT%d" % i, 128, BF16) for i in range(2)]
                        b_PT = [B("mPT0"), B("mPT1")]
                        Cf = sb(s2, "Cf", 129)
                        b_Cf = B("Cf")
                        Ct = sb(s2, "Ct", 129)
                        b_Ct = B("Ct")
                        Cb = [sb(s2, "Cb%d" % i, 129, BF16) for i in range(2)]
                        b_Cb = [B("Cb0"), B("Cb1")]
                        smM = [sb(s2, "sm%d" % i, 8) for i in range(2)]
                        b_smM = [B("sm0"), B("sm1")]
                        hr = [sb(s2, "hr%d" % i, 128) for i in range(2)]
                        b_hr = [B("hr0"), B("hr1")]
                        junkM = [sb(s2, "junk%d" % i, 128) for i in range(2)]
                        b_junkM = [B("junk0"), B("junk1")]
                        hn = [sb(s2, "hn%d" % i, 128, BF16) for i in range(2)]
                        b_hn = [B("hn0"), B("hn1")]
                        V(lambda e: e.memset(qpad[:, 0:3], 0.0), [], [b_qpad])
                        dhs = 128.0 ** -0.5

                        for hd in range(4):
                            def qk_path(chunk, dst, b_dst, scale_k):
                                wq, b_wq = loadw(winF[l, chunk])
                                for t in range(4):
                                    pi = nextps()
                                    proj_fm(wq, b_wq, t, pi)
                                    A(lambda e, pi=pi, t=t: e.activation(out=qpad[:, 3 + t * 512:3 + (t + 1) * 512],
                                                                         in_=ps[pi][:], func=AF.Copy),
                                      [bps[pi]], [b_qpad])
                                ch = chunk - 8
                                w0 = (l * 8 + ch) * 4
                                V(lambda e: e.tensor_scalar(out=cacc[:], in0=qpad[:, 0:T], scalar1=mlcw_s[:, w0:w0 + 1],
                                                            scalar2=mlcb_s[:, l * 8 + ch:l * 8 + ch + 1],
                                                            op0=ALU.mult, op1=ALU.add),
                                  [b_qpad, b_mlcw, b_mlcb], [b_cacc])
                                for jj in range(1, 4):
                                    V(lambda e, jj=jj: e.scalar_tensor_tensor(
                                        out=cacc[:], in0=qpad[:, jj:jj + T], scalar=mlcw_s[:, w0 + jj:w0 + jj + 1],
                                        in1=cacc[:], op0=ALU.mult, op1=ALU.add), [b_qpad, b_mlcw, b_cacc], [b_cacc])
                                if not scale_k:
                                    A(lambda e: e.activation(out=dst[:], in_=cacc[:], func=AF.Silu), [b_cacc], [b_dst])
                                else:
                                    A(lambda e: e.activation(out=cacc[:], in_=cacc[:], func=AF.Silu),
                                      [b_cacc], [b_cacc])
                                    V(lambda e: e.tensor_scalar(out=dst[:], in0=cacc[:], scalar1=dhs, scalar2=None,
                                                                op0=ALU.mult), [b_cacc], [b_dst])

                            qk_path(8 + hd, qT, b_qT, False)
                            qk_path(12 + hd, kT, b_kT, True)
                            for c in range(16):
                                qd = c // 4
                                P(lambda e, c=c: e.transpose(psb[:, c * 128:(c + 1) * 128], kT[:, c * 128:(c + 1) * 128],
                                                             identb[:]), [b_kT, b_identb], [bpsb[qd]], sig=(c % 4 == 3))
                                if c % 4 == 3:
                                    V(lambda e, qd=qd: e.tensor_copy(out=ktm[:, qd * 512:(qd + 1) * 512],
                                                                     in_=psb[:, qd * 512:(qd + 1) * 512]),
                                      [bpsb[qd]], [b_ktm])
                            wv, b_wv = loadw(winT[l, hd])
                            for tt in range(16):
                                pi = nextps()
                                proj_tm(wv, b_wv, tt, ps[pi][:, 0:128], bps[pi])
                                col = tt * 4 + hd
                                V(lambda e, pi=pi, tt=tt, col=col: e.tensor_scalar(
                                    out=va[:, tt * 129:tt * 129 + 128], in0=ps[pi][:, 0:128],
                                    scalar1=AA[:, col:col + 1], scalar2=None, op0=ALU.mult),
                                  [bps[pi], b_g2], [b_va])
                                V(lambda e, tt=tt, col=col: e.tensor_copy(out=va[:, tt * 129 + 128:tt * 129 + 129],
                                                                          in_=AA[:, col:col + 1]), [b_g2], [b_va])
                            wo, b_wo = loadw(winT[l, 4 + hd])
                            for tt in range(16):
                                pi = nextps()
                                proj_tm(wo, b_wo, tt, ps[pi][:, 0:128], bps[pi])
                                A(lambda e, pi=pi, tt=tt: e.activation(out=og[:, tt * 128:(tt + 1) * 128],
                                                                       in_=ps[pi][:, 0:128], func=AF.Sigmoid),
                                  [bps[pi]], [b_og])
                            y_i = yi[0] % 2
                            yi[0] += 1
                            def emitSm(c):
                                cs_ = slice(c * 128, (c + 1) * 128)
                                pS_ = nextps()
                                mm(ps[pS_][:, 0:128], kT[:, cs_], qT[:, cs_], True, True, [b_kT, b_qT], [bps[pS_]])
                                return pS_

                            mpend = []
                            nxtSm = emitSm(0)
                            for c in range(16):
                                cs = slice(c * 128, (c + 1) * 128)
                                col = c * 4 + hd
                                pS = nxtSm
                                if c < 15:
                                    nxtSm = emitSm(c + 1)
                                pt = c % 2
                                sm = smM[c % 2]
                                b_sm = b_smM[c % 2]
                                junk = junkM[c % 2]
                                b_junk = b_junkM[c % 2]
                                V(lambda e, pS=pS, pt=pt: e.tensor_tensor(out=PT[pt][:], in0=ps[pS][:, 0:128],
                                                                          in1=trib[:], op=ALU.mult),
                                  [bps[pS], b_trib], [b_PT[pt]])
                                pO = nextps()
                                mm(ps[pO][:, 0:129], PT[pt][:], va[:, c * 129:(c + 1) * 129], True, c == 0,
                                   [b_PT[pt], b_va], [bps[pO]])
                                if c > 0:
                                    cbp = (c - 1) % 2
                                    mm(ps[pO][:, 0:129], qT[:, cs], Cb[cbp][:], False, True, [b_qT, b_Cb[cbp]],
                                       [bps[pO]])
                                if c < 15:
                                    pU = nextps()
                                    mm(ps[pU][:, 0:129], ktm[:, cs], va[:, c * 129:(c + 1) * 129], True, True,
                                       [b_ktm, b_va], [bps[pU]])
                                    if c == 0:
                                        V(lambda e, pU=pU: e.tensor_copy(out=Ct[:], in_=ps[pU][:, 0:129]),
                                          [bps[pU]], [b_Ct])
                                    else:
                                        V(lambda e, pU=pU: e.tensor_tensor(out=Ct[:], in0=ps[pU][:, 0:129], in1=Cf[:],
                                                                           op=ALU.add), [bps[pU], b_Cf], [b_Ct])
                                    V(lambda e, col=col: e.tensor_scalar(out=Cf[:], in0=Ct[:],
                                                                         scalar1=EBL[:, col:col + 1], scalar2=None,
                                                                         op0=ALU.mult), [b_Ct, b_g2], [b_Cf])
                                    cbn = c % 2
                                    V(lambda e, cbn=cbn: e.tensor_copy(out=Cb[cbn][:], in_=Cf[:]), [b_Cf], [b_Cb[cbn]])
                                hi = c % 2
                                A(lambda e, pO=pO, col=col: e.activation(out=sm[:, 0:1], in_=ps[pO][:, 128:129],
                                                                         func=AF.Abs, scale=EB[:, col:col + 1]),
                                  [bps[pO], b_g2], [b_sm])
                                V(lambda e: e.tensor_scalar(out=sm[:, 1:2], in0=sm[:, 0:1], scalar1=1.0, scalar2=None,
                                                            op0=ALU.max), [b_sm], [b_sm])
                                V(lambda e: e.reciprocal(out=sm[:, 6:7], in_=sm[:, 1:2]), [b_sm], [b_sm])
                                V(lambda e, col=col: e.tensor_tensor(out=sm[:, 2:3], in0=EB[:, col:col + 1],
                                                                     in1=sm[:, 6:7], op=ALU.mult),
                                  [b_sm, b_g2], [b_sm])
                                A(lambda e, pO=pO, hi=hi: e.activation(out=hr[hi][:], in_=ps[pO][:, 0:128],
                                                                       func=AF.Copy, scale=sm[:, 2:3]),
                                  [bps[pO], b_sm], [b_hr[hi]])
                                A(lambda e, hi=hi: e.activation(out=junk[:], in_=hr[hi][:], func=AF.Square,
                                                                accum_out=sm[:, 3:4]), [b_hr[hi]], [b_junk, b_sm])
                                A(lambda e: e.activation(out=sm[:, 4:5], in_=sm[:, 3:4], func=AF.Sqrt, bias=epsc[:],
                                                         scale=1.0 / 128.0), [b_sm, b_epsc], [b_sm])
                                V(lambda e: e.reciprocal(out=sm[:, 5:6], in_=sm[:, 4:5]), [b_sm], [b_sm])
                                V(lambda e, hi=hi, cs=cs: e.scalar_tensor_tensor(
                                    out=hn[hi][:], in0=hr[hi][:], scalar=sm[:, 5:6], in1=og[:, cs], op0=ALU.mult,
                                    op1=ALU.mult), [b_hr[hi], b_sm, b_og], [b_hn[hi]])
                                qd = c % 4

                                def mlate(hi=hi, qd=qd, cs=cs, y_i=y_i, hd=hd):
                                    P(lambda e: e.transpose(psb[:, qd * 512:qd * 512 + 128], hn[hi][:], identb[:]),
                                      [b_hn[hi], b_identb], [bpsb[qd]])
                                    A(lambda e: e.activation(
                                        out=yT[y_i][:, cs], in_=psb[:, qd * 512:qd * 512 + 128], func=AF.Copy,
                                        scale=mlng_s[:, l * 4 + hd:l * 4 + hd + 1]), [bpsb[qd], b_mlng], [b_yT[y_i]])
                                mpend.append(mlate)
                                if len(mpend) > 1:
                                    mpend.pop(0)()
                            while mpend:
                                mpend.pop(0)()
                            store_y(4 + hd, y_i)
                        tk.barrier()

                if "att" in mixers:
                    with ExitStack() as s2:
                        qT = sb(s2, "aqT", T, BF16)
                        b_qT = B("aqT")
                        kT = sb(s2, "akT", T, BF16)
                        b_kT = B("akT")
                        vx = sb(s2, "vx", 16 * 129, BF16)
                        b_vx = B("vx")
                        kmr = sb(s2, "kmr", 8)
                        b_kmr = B("kmr")
                        kmb = sb(s2, "kmb", 8, BF16)
                        b_kmb = B("kmb")
                        gm = sb(s2, "gm", 128)
                        b_gm = B("gm")
                        m8 = sb(s2, "m8", 128)
                        b_m8 = B("m8")
                        sel = sb(s2, "sel", 128)
                        b_sel = B("sel")
                        MBp = sb(s2, "MBp", 160, BF16)
                        b_MBp = B("MBp")
                        R = sb(s2, "R", T, BF16, parts=10)
                        b_R = B("R")
                        PT = [sb(s2, "aPT%d" % i, 512, BF16) for i in range(2)]
                        b_PT = [B("aPT0"), B("aPT1")]
                        smA = [sb(s2, "asm%d" % i, 8) for i in range(2)]
                        b_smA = [B("asm0"), B("asm1")]
                        o_ = [sb(s2, "ao%d" % i, 128) for i in range(2)]
                        b_o = [B("ao0"), B("ao1")]
                        junkA = [sb(s2, "ajunk%d" % i, 128) for i in range(2)]
                        b_junkA = [B("ajunk0"), B("ajunk1")]
                        pend = []
                        fpend = []
                        on = [sb(s2, "aon%d" % i, 128, BF16) for i in range(2)]
                        b_on = [B("aon0"), B("aon1")]
                        oreg = [(2, 0), (3, 0), (4, 0), (5, 0)]
                        b_oreg = [bps[2], bps[3], bps[4], bps[5]]
                        dhs = 128.0 ** -0.5
                        V(lambda e: e.memset(vx[:], 1.0), [], [b_vx])
                        fin = [0]
                        for h in range(8):
                            wq, b_wq = loadw(winF[l, 16 + h])
                            for t in range(4):
                                pi = nextps()
                                proj_fm(wq, b_wq, t, pi)
                                A(lambda e, pi=pi, t=t: e.activation(out=qT[:, t * 512:(t + 1) * 512], in_=ps[pi][:],
                                                                     func=AF.Copy, scale=dhs), [bps[pi]], [b_qT])
                            wk_, b_wk = loadw(winF[l, 24 + h])
                            for t in range(4):
                                pi = nextps()
                                proj_fm(wk_, b_wk, t, pi)
                                A(lambda e, pi=pi, t=t: e.activation(out=kT[:, t * 512:(t + 1) * 512], in_=ps[pi][:],
                                                                     func=AF.Copy), [bps[pi]], [b_kT])
                                V(lambda e, pi=pi, t=t: e.tensor_reduce(
                                    out=kmr[:, 2 * t:2 * t + 2], in_=ps[pi][:].rearrange("p (b s) -> p b s", s=256),
                                    axis=AX.X, op=ALU.add), [bps[pi]], [b_kmr])
                            V(lambda e: e.tensor_scalar(out=kmb[:], in0=kmr[:], scalar1=1.0 / 256.0, scalar2=None,
                                                        op0=ALU.mult), [b_kmr], [b_kmb])
                            wv, b_wv = loadw(winT[l, 8 + h])
                            for tt in range(16):
                                pi = nextps()
                                proj_tm(wv, b_wv, tt, ps[pi][:, 0:128], bps[pi])
                                V(lambda e, pi=pi, tt=tt: e.tensor_copy(out=vx[:, tt * 129:tt * 129 + 128],
                                                                        in_=ps[pi][:, 0:128]), [bps[pi]], [b_vx])
                            pg = nextps()
                            for tt in range(16):
                                mm(ps[pg][:, tt * 8:(tt + 1) * 8], qT[:, tt * 128:(tt + 1) * 128], kmb[:], True, True,
                                   [b_qT, b_kmb], [bps[pg]], sig=(tt == 15))
                            V(lambda e, pg=pg: e.tensor_tensor(out=gm[:], in0=ps[pg][:, 0:128], in1=pastbias[:],
                                                               op=ALU.add), [bps[pg], b_pastbias], [b_gm])
                            for tt in range(16):
                                V(lambda e, tt=tt: e.max(out=m8[:, tt * 8:(tt + 1) * 8], in_=gm[:, tt * 8:(tt + 1) * 8]),
                                  [b_gm], [b_m8], sig=(tt == 15))
                            for tt in range(16):
                                V(lambda e, tt=tt: e.tensor_scalar(out=sel[:, tt * 8:(tt + 1) * 8],
                                                                   in0=gm[:, tt * 8:(tt + 1) * 8],
                                                                   scalar1=m8[:, tt * 8 + 2:tt * 8 + 3], scalar2=None,
                                                                   op0=ALU.is_ge), [b_gm, b_m8], [b_sel],
                                  sig=(tt == 15))
                            V(lambda e: e.tensor_tensor(out=sel[:], in0=sel[:], in1=past01[:], op=ALU.mult),
                              [b_sel, b_past01], [b_sel])
                            V(lambda e: e.tensor_tensor(out=sel[:], in0=sel[:], in1=own01[:], op=ALU.add),
                              [b_sel, b_own01], [b_sel])
                            mb3 = MBp[:].rearrange("p (t c) -> p t c", c=10)
                            V(lambda e: e.tensor_scalar(out=mb3[:, :, 2:10],
                                                        in0=sel[:].rearrange("p (t c) -> p t c", c=8), scalar1=-1.0,
                                                        scalar2=BIG, op0=ALU.add, op1=ALU.mult), [b_sel], [b_MBp])
                            V(lambda e, h=h: e.tensor_copy(
                                out=mb3[:, :, 0:2],
                                in_=alibi[:, h * 32:(h + 1) * 32].rearrange("p (t c) -> p t c", c=2)),
                              [b_alibi], [b_MBp])
                            for tt in range(16):
                                qd = tt // 4
                                P(lambda e, tt=tt: e.transpose(psb[0:10, tt * 128:(tt + 1) * 128],
                                                               MBp[:, tt * 10:(tt + 1) * 10], identb[:]),
                                  [b_MBp, b_identb], [bpsb[qd]], sig=(tt % 4 == 3))
                            A(lambda e: e.activation(out=R[:], in_=psb[0:10, :], func=AF.Copy), bpsb, [b_R])
                            y_i = yi[0] % 2
                            yi[0] += 1
                            its = [(g, kt) for g in range(4) for kt in range(4 * g + 4)]

                            def emitS(g, kt):
                                off = max(0, kt - 4 * g) * 128
                                N = 512 - off
                                q0 = g * 512 + off
                                n = kt // 2
                                pS = nextps(2)
                                mm(ps[pS][:, 0:N], kT[:, kt * 128:(kt + 1) * 128], qT[:, q0:q0 + N], True, False,
                                   [b_kT, b_qT], [bps[pS]], sig=False)
                                mm(ps[pS][:, 0:N], esel[:, n * 128:(n + 1) * 128], R[:, q0:q0 + N], False, True,
                                   [b_esel, b_R], [bps[pS]])
                                return pS

                            nxtS = emitS(*its[0])
                            for idx, (g, kt) in enumerate(its):
                                if True:
                                    off = max(0, kt - 4 * g) * 128
                                    N = 512 - off
                                    pS = nxtS
                                    if idx + 1 < len(its):
                                        nxtS = emitS(*its[idx + 1])
                                    while pend:
                                        pend.pop(0)()
                                    if kt == 0:
                                        while fpend:
                                            fpend.pop(0)()
                                        for bk_ in (2, 3, 4, 5):
                                            V(lambda e, bk_=bk_: e.memset(ps[bk_][:, 0:129], 0.0), [], [bps[bk_]])
                                    pt = fin[0] % 2
                                    fin[0] += 1
                                    A(lambda e, pS=pS, N=N, pt=pt, kt=kt, h=h: e.activation(
                                        out=PT[pt][:, 0:N], in_=ps[pS][:, 0:N], func=AF.Exp,
                                        bias=kpb[:, h * 16 + kt:h * 16 + kt + 1], scale=1.0),
                                      [bps[pS], b_kpb], [b_PT[pt]])
                                    if kt >= 4 * g:
                                        V(lambda e, pt=pt: e.tensor_tensor(out=PT[pt][:, 0:128], in0=PT[pt][:, 0:128],
                                                                           in1=trib[:], op=ALU.mult),
                                          [b_PT[pt], b_trib], [b_PT[pt]])
                                    for i in range(max(4 * g, kt), 4 * g + 4):
                                        ii = i - 4 * g
                                        col = ii * 128 - off
                                        bk, c0 = oreg[ii]
                                        mm(ps[bk][:, c0:c0 + 129], PT[pt][:, col:col + 128],
                                           vx[:, kt * 129:(kt + 1) * 129], False, kt == i, [b_PT[pt], b_vx],
                                           [b_oreg[ii]], sig=True, skip=True)
                                    while fpend:
                                        fpend.pop(0)()
                                    if kt >= 4 * g:
                                        def finz(kt=kt, g=g, y_i=y_i, h=h):
                                            i = kt
                                            ii = i - 4 * g
                                            bk, c0 = oreg[ii]
                                            oi = i % 2
                                            sm = smA[oi]
                                            b_sm = b_smA[oi]
                                            junk = junkA[oi]
                                            b_junk = b_junkA[oi]
                                            cs = slice(i * 128, (i + 1) * 128)
                                            V(lambda e, bk=bk, c0=c0, sm=sm: e.reciprocal(
                                                out=sm[:, 0:1], in_=ps[bk][:, c0 + 128:c0 + 129]), [b_oreg[ii]], [b_sm])
                                            A(lambda e, bk=bk, c0=c0, oi=oi, sm=sm: e.activation(
                                                out=o_[oi][:], in_=ps[bk][:, c0:c0 + 128], func=AF.Copy, scale=sm[:, 0:1]),
                                              [b_oreg[ii], b_sm], [b_o[oi]])
                                            A(lambda e, oi=oi, sm=sm, junk=junk: e.activation(
                                                out=junk[:], in_=o_[oi][:], func=AF.Square, accum_out=sm[:, 1:2]),
                                              [b_o[oi]], [b_junk, b_sm])
                                            A(lambda e, sm=sm: e.activation(out=sm[:, 2:3], in_=sm[:, 1:2], func=AF.Ln,
                                                                            bias=epsc[:], scale=1.0 / 128.0),
                                              [b_sm, b_epsc], [b_sm])
                                            A(lambda e, sm=sm: e.activation(out=sm[:, 3:4], in_=sm[:, 2:3], func=AF.Exp,
                                                                            scale=-0.5), [b_sm], [b_sm])
                                            V(lambda e, oi=oi, sm=sm: e.tensor_scalar(out=on[oi][:], in0=o_[oi][:],
                                                                                      scalar1=sm[:, 3:4], scalar2=None,
                                                                                      op0=ALU.mult),
                                              [b_o[oi], b_sm], [b_on[oi]])
                                            qd = i % 4

                                            def late(oi=oi, qd=qd, cs=cs, y_i=y_i, h=h):
                                                P(lambda e: e.transpose(psb[:, qd * 512:qd * 512 + 128], on[oi][:],
                                                                        identb[:]), [b_on[oi], b_identb], [bpsb[qd]])
                                                A(lambda e: e.activation(
                                                    out=yT[y_i][:, cs], in_=psb[:, qd * 512:qd * 512 + 128], func=AF.Copy,
                                                    scale=atng_s[:, l * 8 + h:l * 8 + h + 1]), [bpsb[qd], b_atng],
                                                  [b_yT[y_i]])
                                            pend.append(late)
                                        fpend.append(finz)
                            while fpend:
                                fpend.pop(0)()
                            while pend:
                                pend.pop(0)()
                            store_y(8 + h, y_i)
                        tk.barrier()
                tk.barrier()

        def phase_outproj(l, src):
            with ExitStack() as st:
                wbs = [sb(st, "wo%d" % i, 2048, BF16) for i in range(2)]
                b_wbs = [B("wo0"), B("wo1")]
                xk = [sb(st, "oxk%d" % i, T) for i in range(2)]
                b_xk = [B("oxk0"), B("oxk1")]
                xo = [sb(st, "oxo%d" % i, T) for i in range(2)]
                b_xo = [B("oxo0"), B("oxo1")]
                for k in range(16):
                    tk.dma("sp", lambda e, k=k: e.dma_start(out=ht(k, 0, T), in_=YT[k * 128:(k + 1) * 128, :]),
                           b_HT[k], reads=[b_YT[k]], writes=[b_HT[k]])
                for m in range(16):
                    i = m % 2
                    tk.dma("pool", lambda e, m=m, i=i: e.dma_start(out=wbs[i][:], in_=wout[l, m]), b_wbs[i],
                           writes=[b_wbs[i]])
                    tk.dma("sp", lambda e, m=m, i=i: e.dma_start(out=xk[i][:], in_=src[m * 128:(m + 1) * 128, :]),
                           b_xk[i], reads=[b_XT[m]], writes=[b_xk[i]])
                    for t in range(4):
                        pi = nextps()
                        proj_fm(wbs[i], b_wbs[i], t, pi)
                        sl = slice(t * 512, (t + 1) * 512)
                        V(lambda e, pi=pi, sl=sl, i=i, m=m: e.scalar_tensor_tensor(
                            out=xo[i][:, sl], in0=ps[pi][:], scalar=mcol(l, 2, m), in1=xk[i][:, sl], op0=ALU.mult,
                            op1=ALU.add), [bps[pi], b_modT, b_xk[i]], [b_xo[i]])
                    tk.dma("sp", lambda e, m=m, i=i: e.dma_start(out=XT[m * 128:(m + 1) * 128, :], in_=xo[i][:]),
                           b_xo[i], reads=[b_xo[i]], writes=[b_XT[m]])
                tk.barrier()

        def phase_ffn(l, src, last):
            moe = (l % 2 == 1)
            li = l // 2
            nexp = moe_experts if moe else 1
            with ExitStack() as st:
                xt = sb(st, "xt", 16 * 512)
                b_xt = [B("xt%d" % k) for k in range(16)]
                h2s = [sb(st, "h2%s" % c_, 16 * 512, BF16) for c_ in "ab"]
                b_h2s = [[B("h2%s_%d" % (c_, k)) for k in range(16)] for c_ in "ab"]
                act = sb(st, "act", NJ * 512, BF16)
                b_act = [B("act%d" % j) for j in range(NJ)]
                wgb = [sb(st, "wgb%d" % i, 2048, BF16) for i in range(2)]
                b_wgb = [B("wgb0"), B("wgb1")]
                wub = [sb(st, "wub%d" % i, 2048, BF16) for i in range(2)]
                b_wub = [B("wub0"), B("wub1")]
                wdb = [sb(st, "wdb%d" % i, NJ * 128, BF16) for i in range(2)]
                b_wdb = [B("wdb0"), B("wdb1")]
                sqt = [sb(st, "fsq%d" % i, 512, BF16) for i in range(2)]
                b_sqt = [B("fsq0"), B("fsq1")]
                rsB = sb(st, "frsB", 512)
                b_rsB = B("frsB")
                tmp = [sb(st, "ftmp%d" % i, 512) for i in range(2)]
                b_tmp = [B("ftmp0"), B("ftmp1")]
                sgt = [sb(st, "fsg%d" % i, 512) for i in range(2)]
                b_sgt = [B("fsg0"), B("fsg1")]
                if moe:
                    h2f = [sb(st, "h2f%d" % i, 512) for i in range(2)]
                    b_h2f = [B("h2f0"), B("h2f1")]
                    lg = sb(st, "lg", 32)
                    b_lg = B("lg")
                    m8 = sb(st, "fm8", 32)
                    b_m8 = B("fm8")
                    rsel = sb(st, "rsel", 32)
                    b_rsel = B("rsel")
                    ex = sb(st, "ex", 32)
                    b_ex = B("ex")
                    rsm = sb(st, "rsm", 16)
                    b_rsm = B("rsm")
                    GT = sb(st, "GT", 32, BF16)
                    b_GT = B("GT")
                    GR = sb(st, "GR", 512, BF16, parts=8)
                    b_GR = B("GR")
                    gB = [sb(st, "gB%d" % i, 512) for i in range(2)]
                    b_gB = [B("gB0"), B("gB1")]
                wcnt = [0, 0]

                def stats(pn):
                    for k in range(16):
                        i = k % 2
                        A(lambda e, k=k, i=i: e.activation(out=sqt[i][:], in_=xt[:, k * 512:(k + 1) * 512],
                                                           func=AF.Square), [b_xt[k]], [b_sqt[i]])
                        mm(ps[pn][:], onesb[:], sqt[i][:], k == 0, k == 15, [b_onesb, b_sqt[i]], [bps[pn]], sig=True)
                    A(lambda e: e.activation(out=rsB[:], in_=ps[pn][:], func=AF.Sqrt, bias=epsc[:], scale=1.0 / D),
                      [bps[pn], b_epsc], [b_rsB])
                    V(lambda e: e.reciprocal(out=rsB[:], in_=rsB[:]), [b_rsB], [b_rsB])

                stg = [sb(st, "stg%d" % i, 512) for i in range(2)]
                b_stg = [B("stg0"), B("stg1")]
                pR = 5

                def prologue(tI):
                    par = tI % 2
                    tsl = slice(tI * 512, (tI + 1) * 512)
                    h2p = h2s[par]
                    b_h2p = b_h2s[par]
                    for k in range(16):
                        i = k % 2
                        tk.dma("sp", lambda e, k=k, i=i: e.dma_start(out=stg[i][:], in_=src[k * 128:(k + 1) * 128, tsl]),
                               b_stg[i], reads=[b_XT[k]], writes=[b_stg[i]])
                        A(lambda e, i=i: e.activation(out=sqt[i][:], in_=stg[i][:], func=AF.Square),
                          [b_stg[i]], [b_sqt[i]])
                        mm(ps[4][:], onesb[:], sqt[i][:], k == 0, k == 15, [b_onesb, b_sqt[i]], [bps[4]], sig=True)
                    A(lambda e: e.activation(out=rsB[:], in_=ps[4][:], func=AF.Sqrt, bias=epsc[:], scale=1.0 / D),
                      [bps[4], b_epsc], [b_rsB])
                    V(lambda e: e.reciprocal(out=rsB[:], in_=rsB[:]), [b_rsB], [b_rsB])
                    if moe:
                        V(lambda e: e.memset(ps[pR][:, 0:32], 0.0), [], [bps[pR]])
                    for k in range(16):
                        i = k % 2
                        tk.dma("sp", lambda e, k=k, i=i: e.dma_start(out=stg[i][:], in_=src[k * 128:(k + 1) * 128, tsl]),
                               b_stg[i], reads=[b_XT[k]], writes=[b_stg[i]])
                        V(lambda e, k=k, i=i: e.tensor_tensor(out=tmp[i][:], in0=stg[i][:], in1=rsB[:], op=ALU.mult),
                          [b_stg[i], b_rsB], [b_tmp[i]])
                        A(lambda e, k=k, i=i: e.activation(out=h2p[:, k * 512:(k + 1) * 512], in_=tmp[i][:],
                                                           func=AF.Identity, bias=mcol(l, 3, k),
                                                           scale=A2[:, l * 16 + k:l * 16 + k + 1]),
                          [b_tmp[i], b_modT, b_A], [b_h2p[k]])
                        if moe:
                            A(lambda e, k=k, i=i: e.activation(out=h2f[i][:], in_=tmp[i][:], func=AF.Identity,
                                                               bias=mcol(l, 3, k),
                                                               scale=A2[:, l * 16 + k:l * 16 + k + 1]),
                              [b_tmp[i], b_modT, b_A], [b_h2f[i]])
                            for tq in range(4):
                                c0 = (li * 16 + k) * 8
                                P(lambda e, tq=tq, i=i, c0=c0, k=k: e.matmul(
                                    ps[pR][:, tq * 8:(tq + 1) * 8], h2f[i][:, tq * 128:(tq + 1) * 128],
                                    rw_s[:, c0:c0 + 8], start=False, stop=(k == 15), skip_group_check=True),
                                  [b_h2f[i], b_rw], [bps[pR]], tq == 3)
                    if moe:
                        V(lambda e: e.tensor_tensor(out=lg[:], in0=ps[pR][:, 0:32], in1=rb_s[:, li * 32:(li + 1) * 32],
                                                    op=ALU.add), [bps[pR], b_rb], [b_lg])
                        for tq in range(4):
                            s8 = slice(tq * 8, (tq + 1) * 8)
                            V(lambda e, s8=s8: e.max(out=m8[:, s8], in_=lg[:, s8]), [b_lg], [b_m8])
                            V(lambda e, s8=s8, tq=tq: e.tensor_scalar(out=rsel[:, s8], in0=lg[:, s8],
                                                                      scalar1=m8[:, tq * 8 + 1:tq * 8 + 2],
                                                                      scalar2=None, op0=ALU.is_ge),
                              [b_lg, b_m8], [b_rsel])
                            V(lambda e, tq=tq: e.tensor_scalar(out=rsm[:, tq:tq + 1], in0=m8[:, tq * 8:tq * 8 + 1],
                                                               scalar1=-1.0, scalar2=None, op0=ALU.mult),
                              [b_m8], [b_rsm])
                            A(lambda e, s8=s8, tq=tq: e.activation(out=ex[:, s8], in_=lg[:, s8], func=AF.Exp,
                                                                   bias=rsm[:, tq:tq + 1], scale=1.0),
                              [b_lg, b_rsm], [b_ex])
                            V(lambda e, s8=s8: e.tensor_tensor(out=ex[:, s8], in0=ex[:, s8], in1=rsel[:, s8],
                                                               op=ALU.mult), [b_ex, b_rsel], [b_ex])
                            V(lambda e, s8=s8, tq=tq: e.tensor_reduce(out=rsm[:, 4 + tq:5 + tq], in_=ex[:, s8],
                                                                      axis=AX.X, op=ALU.add), [b_ex], [b_rsm])
                            V(lambda e, tq=tq: e.reciprocal(out=rsm[:, 8 + tq:9 + tq], in_=rsm[:, 4 + tq:5 + tq]),
                              [b_rsm], [b_rsm])
                            V(lambda e, s8=s8, tq=tq: e.tensor_scalar(out=GT[:, s8], in0=ex[:, s8],
                                                                      scalar1=rsm[:, 8 + tq:9 + tq], scalar2=None,
                                                                      op0=ALU.mult), [b_ex, b_rsm], [b_GT])
                        for tq in range(4):
                            P(lambda e, tq=tq: e.transpose(psb[0:8, tq * 128:(tq + 1) * 128],
                                                           GT[:, tq * 8:(tq + 1) * 8], identb[:]),
                              [b_GT, b_identb], [bpsb[0]], tq == 3)
                        A(lambda e: e.activation(out=GR[:], in_=psb[0:8, 0:512], func=AF.Copy), [bpsb[0]], [b_GR])


                prologue(0)
                for tI in range(4):
                    tsl = slice(tI * 512, (tI + 1) * 512)
                    h2 = h2s[tI % 2]
                    b_h2 = b_h2s[tI % 2]
                    for k in range(16):
                        tk.dma("sp", lambda e, k=k: e.dma_start(out=xt[:, k * 512:(k + 1) * 512],
                                                                in_=src[k * 128:(k + 1) * 128, tsl]),
                               b_xt[k], reads=[b_XT[k]], writes=[b_xt[k]])
                    for ex_i in range(nexp):
                        if moe:
                            gi = ex_i % 2
                            pgb = nextps()
                            mm(ps[pgb][:], e8[:, ex_i * 128:(ex_i + 1) * 128], GR[:], True, True, [b_e8, b_GR],
                               [bps[pgb]])
                            A(lambda e, gi=gi, pgb=pgb: e.activation(out=gB[gi][:], in_=ps[pgb][:], func=AF.Copy),
                              [bps[pgb]], [b_gB[gi]])
                        for j in range(NJ):
                            i = wcnt[0] % 2
                            wcnt[0] += 1
                            gsrc = mog[li, ex_i, j] if moe else ffg[li, j]
                            usrc = mou[li, ex_i, j] if moe else ffu[li, j]
                            tk.dma("pool", lambda e, i=i, gsrc=gsrc: e.dma_start(out=wgb[i][:], in_=gsrc), b_wgb[i],
                                   writes=[b_wgb[i]])
                            tk.dma("pool", lambda e, i=i, usrc=usrc: e.dma_start(out=wub[i][:], in_=usrc), b_wub[i],
                                   writes=[b_wub[i]])
                            pg = nextps()
                            for k in range(16):
                                mm(ps[pg][:], wgb[i][:, k * 128:(k + 1) * 128], h2[:, k * 512:(k + 1) * 512], k == 0,
                                   k == 15, [b_wgb[i], b_h2[k]], [bps[pg]])
                            pu = nextps()
                            for k in range(16):
                                mm(ps[pu][:], wub[i][:, k * 128:(k + 1) * 128], h2[:, k * 512:(k + 1) * 512], k == 0,
                                   k == 15, [b_wub[i], b_h2[k]], [bps[pu]])
                            si = j % 2
                            A(lambda e, si=si, pg=pg: e.activation(out=sgt[si][:], in_=ps[pg][:], func=AF.Silu),
                              [bps[pg]], [b_sgt[si]])
                            V(lambda e, si=si, pu=pu, j=j: e.tensor_tensor(out=act[:, j * 512:(j + 1) * 512],
                                                                           in0=sgt[si][:], in1=ps[pu][:],
                                                                           op=ALU.mult),
                              [b_sgt[si], bps[pu]], [b_act[j]])
                        if ex_i == nexp - 1 and tI < 3:
                            prologue(tI + 1)
                        for m in range(16):
                            i = wcnt[1] % 2
                            wcnt[1] += 1
                            dsrc = mod_[li, ex_i, m] if moe else ffd[li, m]
                            tk.dma("pool", lambda e, i=i, dsrc=dsrc: e.dma_start(out=wdb[i][:], in_=dsrc), b_wdb[i],
                                   writes=[b_wdb[i]])
                            pd = nextps()
                            for j in range(NJ):
                                mm(ps[pd][:], wdb[i][:, j * 128:(j + 1) * 128], act[:, j * 512:(j + 1) * 512], j == 0,
                                   j == NJ - 1, [b_wdb[i], b_act[j]], [bps[pd]])
                            if not moe:
                                V(lambda e, pd=pd, m=m: e.scalar_tensor_tensor(
                                    out=xt[:, m * 512:(m + 1) * 512], in0=ps[pd][:], scalar=mcol(l, 5, m),
                                    in1=xt[:, m * 512:(m + 1) * 512], op0=ALU.mult, op1=ALU.add),
                                  [bps[pd], b_modT, b_xt[m]], [b_xt[m]])
                            else:
                                ti = m % 2
                                V(lambda e, pd=pd, m=m, ti=ti, gi=gi: e.scalar_tensor_tensor(
                                    out=tmp[ti][:], in0=ps[pd][:], scalar=mcol(l, 5, m), in1=gB[gi][:],
                                    op0=ALU.mult, op1=ALU.mult), [bps[pd], b_modT, b_gB[gi]], [b_tmp[ti]])
                                V(lambda e, m=m, ti=ti: e.tensor_tensor(
                                    out=xt[:, m * 512:(m + 1) * 512], in0=xt[:, m * 512:(m + 1) * 512],
                                    in1=tmp[ti][:], op=ALU.add), [b_xt[m], b_tmp[ti]], [b_xt[m]])
                    if not last:
                        for k in range(16):
                            tk.dma("sp", lambda e, k=k: e.dma_start(out=XT[k * 128:(k + 1) * 128, tsl],
                                                                    in_=xt[:, k * 512:(k + 1) * 512]),
                                   b_xt[k], reads=[b_xt[k]], writes=[b_XT[k]])
                    else:
                        stats(4)
                        for k in range(16):
                            i = k % 2
                            V(lambda e, k=k, i=i: e.scalar_tensor_tensor(
                                out=tmp[i][:], in0=xt[:, k * 512:(k + 1) * 512], scalar=gfin_s[:, k:k + 1], in1=rsB[:],
                                op0=ALU.mult, op1=ALU.mult), [b_xt[k], b_gfin, b_rsB], [b_tmp[i]])
                            tk.dma("sp", lambda e, k=k, i=i: e.dma_start(out=outT[k * 128:(k + 1) * 128, tsl],
                                                                         in_=tmp[i][:]),
                                   b_tmp[i], reads=[b_tmp[i]], writes=[b_XT[k]])
                tk.barrier()

        src = xT_in
        for l in range(nl):
            if do_mix:
                with ExitStack() as sth:
                    HTh[0] = sb(sth, "HT", 16 * T, BF16)
                    phase_norm1(l, src)
                    phase_mixer(l)
                    phase_outproj(l, src)
                    tk.barrier()
                src = XT
            if do_ffn:
                phase_ffn(l, src, last=(l == nl - 1))
                src = XT
        tk.barrier()
        build_program.stats = dict(tk.ninst)
    return nc


def _ptab(v, nchunk):
    return np.ascontiguousarray(np.asarray(v, np.float32).reshape(nchunk, 128).T)


def _wtile(w, cols):
    K, n = w.shape
    nb = n // cols
    kc = K // 128
    return np.ascontiguousarray(w.reshape(kc, 128, nb, cols).transpose(2, 1, 0, 3).reshape(nb, 128, kc * cols))


def _constants():
    bf = ml_dtypes.bfloat16
    c = {}
    c["c_identb"] = np.eye(128, dtype=np.float32).astype(bf)
    c["c_ones"] = np.ones((128, 128), np.float32)
    tri = np.triu(np.ones((128, 128), np.float32))
    c["c_tri"] = tri
    c["c_trib"] = tri.astype(bf)
    es = np.zeros((10, 8, 128), np.float32)
    es[0, :, :] = 1.0
    es[1, :, :] = 1.0
    for n in range(8):
        es[2 + n, n, :] = 1.0
    c["c_esel"] = es.reshape(10, 8 * 128).astype(bf)
    e8 = np.zeros((8, 8, 128), np.float32)
    for n in range(8):
        e8[n, n, :] = 1.0
    c["c_e8"] = e8.reshape(8, 8 * 128).astype(bf)
    slopes = 2.0 ** (-(np.arange(1, 9, dtype=np.float64)))
    pos = np.arange(2048, dtype=np.float64).reshape(16, 128)
    al = np.zeros((128, 8, 16, 2), np.float32)
    for h in range(8):
        v = (-slopes[h] * pos).T
        hi = v.astype(np.float32).astype(bf)
        lo = (v - hi.astype(np.float64)).astype(np.float32).astype(bf)
        al[:, h, :, 0] = hi.astype(np.float32)
        al[:, h, :, 1] = lo.astype(np.float32)
    c["c_alibi"] = al.reshape(128, 8 * 16 * 2).astype(bf)
    kp = np.zeros((128, 8, 16), np.float32)
    for h in range(8):
        kp[:, h, :] = (slopes[h] * pos).T
    c["c_kpb"] = kp.reshape(128, 128)
    pb = np.zeros((128, 16, 8), np.float32)
    p01 = np.zeros((128, 16, 8), np.float32)
    o01 = np.zeros((128, 16, 8), np.float32)
    for tt in range(16):
        qb = tt // 2
        for n in range(8):
            if n < qb:
                p01[:, tt, n] = 1.0
            else:
                pb[:, tt, n] = -1e30
            if n == qb:
                o01[:, tt, n] = 1.0
    c["c_pastbias"] = pb.reshape(128, 128)
    c["c_past01"] = p01.reshape(128, 128)
    c["c_own01"] = o01.reshape(128, 128)
    return c


def prep_shared(inp):
    f = lambda a: np.asarray(a, np.float32)
    sh = {}
    w_mod = f(inp["w_mod"])
    sh["wmod"] = np.ascontiguousarray(
        w_mod.reshape(NL, 16, 128, 24, 512).transpose(0, 3, 2, 1, 4).reshape(NL, 24, 128, 16 * 512))
    sh["bmodT"] = np.concatenate([_ptab(f(inp["b_mod"])[l], 96) for l in range(NL)], axis=1)
    sh["gmixT"] = np.concatenate([_ptab(f(inp["g_mix"])[l], 16) for l in range(NL)], axis=1)
    sh["gffnT"] = np.concatenate([_ptab(f(inp["g_ffn"])[l], 16) for l in range(NL)], axis=1)
    sh["gfinT"] = _ptab(f(inp["g_final"]), 16)
    w_in = f(inp["w_in"])
    fm_cols = np.concatenate([np.arange(0, 2048), np.arange(3080, 5128)])
    tm_cols = np.concatenate([np.arange(2048, 3072), np.arange(5128, 6152)])
    sh["winF"] = np.stack([_wtile(np.ascontiguousarray(w_in[l][:, fm_cols]), 128) for l in range(NL)])
    sh["winT"] = np.stack([_wtile(np.ascontiguousarray(w_in[l][:, tm_cols]), 128) for l in range(NL)])
    sh["winG"] = np.stack([_wtile(np.ascontiguousarray(w_in[l][:, 3072:3080]), 8)[0] for l in range(NL)])
    cw = f(inp["conv_w"])
    sh["convw"] = np.ascontiguousarray(
        cw.reshape(NL, 31, 4, 128).transpose(3, 0, 2, 1).reshape(128, NL * 4 * 31))
    cp = np.stack([f(inp["conv_b"]), f(inp["conv_ln_g"]), f(inp["conv_ln_b"])], axis=-1)
    sh["convp"] = np.ascontiguousarray(cp.reshape(NL, 4, 128, 3).transpose(2, 0, 1, 3).reshape(128, NL * 4 * 3))
    mw = f(inp["ml_conv_w"])
    sh["mlcw"] = np.ascontiguousarray(mw.reshape(NL, 4, 8, 128).transpose(3, 0, 2, 1).reshape(128, NL * 8 * 4))
    sh["mlcb"] = np.ascontiguousarray(f(inp["ml_conv_b"]).reshape(NL, 8, 128).transpose(2, 0, 1).reshape(128, NL * 8))
    bif = np.concatenate([f(inp["ml_b_i"]), f(inp["ml_b_f"])], axis=1)
    sh["mlbif"] = np.ascontiguousarray(np.broadcast_to(bif[None, :, None, :], (128, NL, 16, 8)).reshape(128, NL * 128))
    sh["mlng"] = np.ascontiguousarray(f(inp["ml_norm_g"]).reshape(NL, 4, 128).transpose(2, 0, 1).reshape(128, NL * 4))
    sh["atng"] = np.ascontiguousarray(f(inp["attn_norm_g"]).reshape(NL, 8, 128).transpose(2, 0, 1).reshape(128, NL * 8))
    sh["wout"] = np.stack([_wtile(f(inp["w_out"])[l], 128) for l in range(NL)])
    sh["ffg"] = np.stack([_wtile(f(inp["ffn_w_gate"])[i], 128) for i in range(2)])
    sh["ffu"] = np.stack([_wtile(f(inp["ffn_w_up"])[i], 128) for i in range(2)])

    def dtile(w):
        return np.ascontiguousarray(w.reshape(NJ, 128, 16, 128).transpose(2, 1, 0, 3).reshape(16, 128, NJ * 128))

    sh["ffd"] = np.stack([dtile(f(inp["ffn_w_down"])[i]) for i in range(2)])
    sh["mog"] = np.stack([np.stack([_wtile(f(inp["moe_w_gate"])[i, e], 128) for e in range(NE)]) for i in range(2)])
    sh["mou"] = np.stack([np.stack([_wtile(f(inp["moe_w_up"])[i, e], 128) for e in range(NE)]) for i in range(2)])
    sh["mod"] = np.stack([np.stack([dtile(f(inp["moe_w_down"])[i, e]) for e in range(NE)]) for i in range(2)])
    rwt = f(inp["moe_w_router"])
    sh["rw"] = np.ascontiguousarray(rwt.reshape(2, 16, 128, 8).transpose(2, 0, 1, 3).reshape(128, 2 * 16 * 8))
    rbt = f(inp["moe_b_router"])
    sh["rb"] = np.ascontiguousarray(np.broadcast_to(rbt[None, :, None, :], (128, 2, 4, 8)).reshape(128, 64))
    sh.update(_constants())
    return sh


def kernel(**inputs):
    x = np.asarray(inputs["x"], np.float32)
    c = np.asarray(inputs["c"], np.float32)
    B = x.shape[0]
    sh = prep_shared(inputs)
    nc = build_program()
    in_maps = []
    for b in range(B):
        m = dict(sh)
        m["xT"] = np.ascontiguousarray(x[b].T)
        m["condT"] = _ptab(c[b], 16)
        in_maps.append(m)
    res = run_bass_kernel_spmd(nc, in_maps, core_ids=list(range(B)))
    out = np.stack([np.ascontiguousarray(np.asarray(r["outT"], np.float32).T) for r in res.results])
    return out
```
